# Optimizing a Trainium2 kernel written in Bass

```python
import math
import jax, jax.numpy as jnp
from jax import lax
import numpy as np

D_MODEL = 4096
BATCH = 2
SEQ = 8192
DEPTH = 1

D_MIX = D_MODEL
SB_HEADS = 16
SB_HEAD_DIM = 128
SB_WIDTH = SB_HEADS * SB_HEAD_DIM
ML_HEADS = 4
ML_HEAD_DIM = 512
ML_WIDTH = ML_HEADS * ML_HEAD_DIM
CONV_WIDTH = 4
ML_CHUNK = 64
Q_BLOCK = 128
PROJ_OUT = 3 * SB_WIDTH + 4 * ML_WIDTH + 2 * ML_HEADS
N_GROUPS = 4
EXPERTS_PER_GROUP = 8
N_EXPERTS = N_GROUPS * EXPERTS_PER_GROUP
TOP_K = 2
D_FF_EXPERT = 1024
MOE_BLOCK = 256
NORM_EPS = 1e-6
N_LAYER_MOD = 6
N_FINAL_MOD = 2

kernel_name = "hybrid_stickbreak_mlstm_hmoe_block"


def rms_norm(x, w):
    xf = x.astype(jnp.float32)
    y = xf * lax.rsqrt(jnp.mean(xf * xf, axis=-1, keepdims=True) + NORM_EPS)
    return (y * w.astype(jnp.float32)).astype(x.dtype)


def modulate(h, shift, scale):
    return h * (1.0 + scale[:, None, :]) + shift[:, None, :]


def stick_breaking_attention(q, k, v):
    b, s, h, d = q.shape
    n_blk = s // Q_BLOCK
    scale = 1.0 / math.sqrt(d)
    qb = q.reshape(b, n_blk, Q_BLOCK, h, d).transpose(1, 0, 3, 2, 4)
    kf = k.astype(jnp.float32)
    vf = v.astype(jnp.float32)
    key_pos = jnp.arange(s)

    def block(args):
        q_blk, blk_idx = args
        z = jnp.einsum('bhqd,bkhd->bhqk', q_blk.astype(jnp.float32), kf) * scale
        q_pos = blk_idx * Q_BLOCK + jnp.arange(Q_BLOCK)
        causal = key_pos[None, :] < q_pos[:, None]
        log_keep = jnp.where(causal, jax.nn.log_sigmoid(-z), 0.0)
        between = lax.cumsum(log_keep, axis=3, reverse=True) - log_keep
        w = jnp.where(causal, jnp.exp(jax.nn.log_sigmoid(z) + between), 0.0)
        return jnp.einsum('bhqk,bkhd->bqhd', w, vf)

    out = lax.map(block, (qb, jnp.arange(n_blk)))
    return out.transpose(1, 0, 2, 3, 4).reshape(b, s, h, d).astype(q.dtype)


def causal_depthwise_conv(x, w):
    kw, ch = w.shape
    return lax.conv_general_dilated(
        x, w.reshape(kw, 1, ch).astype(x.dtype), window_strides=(1,),
        padding=[(kw - 1, 0)], dimension_numbers=('NWC', 'WIO', 'NWC'),
        feature_group_count=ch)


def mlstm_chunkwise(q, k, v, i_pre, f_pre):
    b, s, h, d = q.shape
    L = ML_CHUNK
    nc = s // L

    def chunks4(t):
        return t.astype(jnp.float32).reshape(b, nc, L, h, d).transpose(1, 0, 3, 2, 4)

    def chunks3(t):
        return t.astype(jnp.float32).reshape(b, nc, L, h).transpose(1, 0, 3, 2)

    qc = chunks4(q)
    kc = chunks4(k) * (1.0 / math.sqrt(d))
    vc = chunks4(v)
    log_i = chunks3(i_pre)
    log_f = jax.nn.log_sigmoid(chunks3(f_pre))
    causal = jnp.tril(jnp.ones((L, L), dtype=bool))

    def step(carry, xs):
        c_st, n_st, m_st = carry
        qk_, kk_, vk_, li, lf = xs
        b_cum = jnp.cumsum(lf, axis=-1)
        a = b_cum + m_st[..., None]
        d_mat = jnp.where(causal, b_cum[..., :, None] - b_cum[..., None, :] + li[..., None, :], -jnp.inf)
        m_t = jnp.maximum(a, jnp.max(d_mat, axis=-1))
        w_intra = jnp.exp(d_mat - m_t[..., None])
        w_inter = jnp.exp(a - m_t)
        sc = jnp.einsum('bhtd,bhsd->bhts', qk_, kk_) * w_intra
        num = (w_inter[..., None] * jnp.einsum('bhtd,bhde->bhte', qk_, c_st)
               + jnp.einsum('bhts,bhse->bhte', sc, vk_))
        den = w_inter * jnp.einsum('bhtd,bhd->bht', qk_, n_st) + jnp.sum(sc, axis=-1)
        h_out = num / jnp.maximum(jnp.abs(den), jnp.exp(-m_t))[..., None]
        b_last = b_cum[..., -1]
        g = b_last[..., None] - b_cum + li
        m_new = jnp.maximum(b_last + m_st, jnp.max(g, axis=-1))
        decay = jnp.exp(b_last + m_st - m_new)
        kw_ = kk_ * jnp.exp(g - m_new[..., None])[..., None]
        c_new = decay[..., None, None] * c_st + jnp.einsum('bhsd,bhse->bhde', kw_, vk_)
        n_new = decay[..., None] * n_st + jnp.sum(kw_, axis=2)
        return (c_new, n_new, m_new), h_out

    init = (jnp.zeros((b, h, d, d), jnp.float32), jnp.zeros((b, h, d), jnp.float32),
            jnp.zeros((b, h), jnp.float32))
    _, hs = lax.scan(step, init, (qc, kc, vc, log_i, log_f))
    return hs.transpose(1, 0, 3, 2, 4).reshape(b, s, h, d)


def hybrid_mixer(u, w_in, conv_w, ml_gate_bias, ml_norm_w, w_out):
    b, s, _ = u.shape
    proj = u @ w_in
    o1 = 3 * SB_WIDTH
    o2 = o1 + 2 * ML_WIDTH
    o3 = o2 + ML_WIDTH
    o4 = o3 + ML_WIDTH
    sb_qkv, ml_qk, ml_v, ml_o, ml_if = jnp.split(proj, [o1, o2, o3, o4], axis=-1)
    sb_q, sb_k, sb_v = [t.reshape(b, s, SB_HEADS, SB_HEAD_DIM) for t in jnp.split(sb_qkv, 3, axis=-1)]
    y_sb = stick_breaking_attention(sb_q, sb_k, sb_v).reshape(b, s, SB_WIDTH)
    ml_qk = jax.nn.silu(causal_depthwise_conv(ml_qk, conv_w))
    ml_q, ml_k = [t.reshape(b, s, ML_HEADS, ML_HEAD_DIM) for t in jnp.split(ml_qk, 2, axis=-1)]
    ml_v = ml_v.reshape(b, s, ML_HEADS, ML_HEAD_DIM)
    ml_if = ml_if + ml_gate_bias
    i_pre, f_pre = jnp.split(ml_if, 2, axis=-1)
    hm = mlstm_chunkwise(ml_q, ml_k, ml_v, i_pre, f_pre)
    hm = hm * lax.rsqrt(jnp.mean(hm * hm, axis=-1, keepdims=True) + NORM_EPS)
    hm = (hm.reshape(b, s, ML_WIDTH) * ml_norm_w.astype(jnp.float32)).astype(u.dtype)
    y_ml = jax.nn.sigmoid(ml_o) * hm
    return jnp.concatenate([y_sb, y_ml], axis=-1) @ w_out


def hierarchical_moe(h, w_router_group, b_router_group, w_router_expert, b_router_expert,
                     w_exp_gate, w_exp_up, w_exp_down):
    n, d = h.shape
    hf = h.astype(jnp.float32)
    g_probs = jax.nn.softmax(hf @ w_router_group.astype(jnp.float32) + b_router_group.astype(jnp.float32), axis=-1)
    g_p, g_sel = lax.top_k(g_probs, 1)
    e_logits = (hf @ w_router_expert.astype(jnp.float32) + b_router_expert.astype(jnp.float32))
    e_logits = e_logits.reshape(n, N_GROUPS, EXPERTS_PER_GROUP)
    e_in_group = jnp.take_along_axis(e_logits, g_sel[:, :, None], axis=1)[:, 0]
    e_probs = jax.nn.softmax(e_in_group, axis=-1)
    top_p, top_local = lax.top_k(e_probs, TOP_K)
    top_w = top_p / jnp.sum(top_p, axis=-1, keepdims=True) * g_p
    top_e = g_sel * EXPERTS_PER_GROUP + top_local

    n_assign = n * TOP_K
    flat_e = top_e.reshape(-1).astype(jnp.int32)
    flat_w = top_w.reshape(-1)
    flat_tok = jnp.repeat(jnp.arange(n, dtype=jnp.int32), TOP_K)
    order = jnp.argsort(flat_e)
    sorted_e = flat_e[order]
    counts = jnp.zeros((N_EXPERTS,), jnp.int32).at[flat_e].add(1)
    padded = (counts + MOE_BLOCK - 1) // MOE_BLOCK * MOE_BLOCK
    starts = jnp.cumsum(counts) - counts
    pends = jnp.cumsum(padded)
    pstarts = pends - padded
    dest = pstarts[sorted_e] + (jnp.arange(n_assign, dtype=jnp.int32) - starts[sorted_e])
    cap = (-(-n_assign // MOE_BLOCK) + N_EXPERTS) * MOE_BLOCK
    n_blocks = cap // MOE_BLOCK
    slot_tok = jnp.full((cap,), n, jnp.int32).at[dest].set(flat_tok[order])
    slot_w = jnp.zeros((cap,), h.dtype).at[dest].set(flat_w[order].astype(h.dtype))
    block_e = jnp.minimum(jnp.searchsorted(pends, jnp.arange(n_blocks, dtype=jnp.int32) * MOE_BLOCK, side='right'),
                          N_EXPERTS - 1).astype(jnp.int32)

    h_pad = jnp.concatenate([h, jnp.zeros((1, d), h.dtype)], axis=0)
    xb = h_pad[slot_tok].reshape(n_blocks, MOE_BLOCK, d)

    def expert_block(args):
        xblk, e = args
        return (jax.nn.silu(xblk @ w_exp_gate[e]) * (xblk @ w_exp_up[e])) @ w_exp_down[e]

    yb = lax.map(expert_block, (xb, block_e)).reshape(cap, d)
    out = jnp.zeros((n + 1, d), h.dtype).at[slot_tok].add(yb * slot_w[:, None])
    return out[:n]


def setup_inputs(seed: int = 0) -> dict:
    key = jax.random.key(seed)
    ks = jax.random.split(key, 24)
    f32 = jnp.float32
    nrm = lambda k, shape, s: jax.random.normal(k, shape, f32) * s
    i_bias = -1.0 + 0.1 * jax.random.normal(ks[4], (DEPTH, ML_HEADS), f32)
    f_bias = jnp.linspace(3.0, 6.0, ML_HEADS, dtype=f32)[None, :] + 0.1 * jax.random.normal(ks[5], (DEPTH, ML_HEADS), f32)
    return {
        "x": nrm(ks[0], (BATCH, SEQ, D_MODEL), 1.0),
        "c": nrm(ks[1], (BATCH, D_MODEL), 1.0),
        "norm1_w": 1.0 + nrm(ks[2], (DEPTH, D_MODEL), 0.02),
        "w_in": nrm(ks[3], (DEPTH, D_MODEL, PROJ_OUT), D_MODEL ** -0.5),
        "conv_w": nrm(ks[6], (DEPTH, CONV_WIDTH, 2 * ML_WIDTH), CONV_WIDTH ** -0.5),
        "ml_gate_bias": jnp.concatenate([i_bias, f_bias], axis=-1),
        "ml_norm_w": 1.0 + nrm(ks[7], (DEPTH, ML_WIDTH), 0.02),
        "w_out": nrm(ks[8], (DEPTH, D_MIX, D_MODEL), D_MIX ** -0.5),
        "norm2_w": 1.0 + nrm(ks[9], (DEPTH, D_MODEL), 0.02),
        "w_router_group": nrm(ks[10], (DEPTH, D_MODEL, N_GROUPS), D_MODEL ** -0.5),
        "b_router_group": nrm(ks[11], (DEPTH, N_GROUPS), 0.01),
        "w_router_expert": nrm(ks[12], (DEPTH, D_MODEL, N_EXPERTS), D_MODEL ** -0.5),
        "b_router_expert": nrm(ks[13], (DEPTH, N_EXPERTS), 0.01),
        "w_exp_gate": nrm(ks[14], (DEPTH, N_EXPERTS, D_MODEL, D_FF_EXPERT), D_MODEL ** -0.5),
        "w_exp_up": nrm(ks[15], (DEPTH, N_EXPERTS, D_MODEL, D_FF_EXPERT), D_MODEL ** -0.5),
        "w_exp_down": nrm(ks[16], (DEPTH, N_EXPERTS, D_FF_EXPERT, D_MODEL), D_FF_EXPERT ** -0.5),
        "w_ada": nrm(ks[17], (DEPTH, D_MODEL, N_LAYER_MOD * D_MODEL), 0.5 * D_MODEL ** -0.5),
        "b_ada": nrm(ks[18], (DEPTH, N_LAYER_MOD * D_MODEL), 0.01),
        "final_norm_w": 1.0 + nrm(ks[19], (D_MODEL,), 0.02),
        "w_ada_final": nrm(ks[20], (D_MODEL, N_FINAL_MOD * D_MODEL), 0.5 * D_MODEL ** -0.5),
        "b_ada_final": nrm(ks[21], (N_FINAL_MOD * D_MODEL,), 0.01),
    }


def reference(x, c, norm1_w, w_in, conv_w, ml_gate_bias, ml_norm_w, w_out, norm2_w,
              w_router_group, b_router_group, w_router_expert, b_router_expert,
              w_exp_gate, w_exp_up, w_exp_down, w_ada, b_ada, final_norm_w,
              w_ada_final, b_ada_final):
    b, s, d = x.shape
    c_act = jax.nn.silu(c)
    h = x
    for layer in range(DEPTH):
        mod = c_act @ w_ada[layer] + b_ada[layer]
        sh1, sc1, g1, sh2, sc2, g2 = jnp.split(mod, N_LAYER_MOD, axis=-1)
        u = modulate(rms_norm(h, norm1_w[layer]), sh1, sc1)
        mix = hybrid_mixer(u, w_in[layer], conv_w[layer], ml_gate_bias[layer], ml_norm_w[layer], w_out[layer])
        h = h + g1[:, None, :] * mix
        u = modulate(rms_norm(h, norm2_w[layer]), sh2, sc2)
        ffn = hierarchical_moe(u.reshape(b * s, d), w_router_group[layer], b_router_group[layer],
                               w_router_expert[layer], b_router_expert[layer],
                               w_exp_gate[layer], w_exp_up[layer], w_exp_down[layer]).reshape(b, s, d)
        h = h + g2[:, None, :] * ffn
    fmod = c_act @ w_ada_final + b_ada_final
    sh_f, sc_f = jnp.split(fmod, N_FINAL_MOD, axis=-1)
    return modulate(rms_norm(h, final_norm_w), sh_f, sc_f)
```

```python
import os as _os
import numpy as np
from contextlib import ExitStack
import concourse.bass as bass
import concourse.mybir as mybir
from concourse.bass_utils import run_bass_kernel_spmd

F32 = mybir.dt.float32
BF16 = mybir.dt.bfloat16
AF = mybir.ActivationFunctionType
ALU = mybir.AluOpType
AX = mybir.AxisListType

D = 4096
KC = 32
NSLOT = 16
SL = 512
OWN = [3, 7, 11, 15]
NEXP = 32
DFF = 1024
EPS = 1e-6
PROJ = 14344


class Buf:
    __slots__ = ("name", "w", "r")

    def __init__(self, name):
        self.name = name
        self.w = None
        self.r = []


class Eng:
    def __init__(self, name, eng, sem):
        self.name = name
        self.eng = eng
        self.sem = sem
        self.count = 0
        self.ins = []
        self.waited = {}


class Sched:
    def __init__(self, nc, es, ndma=12):
        self.nc = nc
        self.E = {}
        for nm, e in (("pe", nc.tensor), ("act", nc.scalar), ("dve", nc.vector), ("pool", nc.gpsimd), ("sp", nc.sync)):
            self.E[nm] = Eng(nm, e, es.enter_context(nc.semaphore("sem_" + nm)))
        self.dsem = {}
        for q in ("sp", "pool", "act"):
            self.dsem[q] = [[es.enter_context(nc.semaphore("dq_%s_%d" % (q, i))), 0] for i in range(ndma)]
        self.dnext = {"sp": 0, "pool": 0, "act": 0}
        self.bufs = {}
        self.all_dma_tokens = []

    def B(self, *key):
        b = self.bufs.get(key)
        if b is None:
            b = Buf(key)
            self.bufs[key] = b
        return b

    def _resolve(self, tok):
        if tok[0] == "sem":
            return tok[1], tok[2]
        e, idx = tok[1], tok[2]
        for k in range(idx, len(e.ins)):
            if e.ins[k][1] is not None:
                return e.sem, e.ins[k][1]
        last = e.ins[-1]
        e.count += 1
        last[0].then_inc(e.sem, 1)
        last[1] = e.count
        return e.sem, e.count

    def _wait(self, eng, toks):
        need = {}
        for t in toks:
            if t is None:
                continue
            sem, val = self._resolve(t)
            k = id(sem)
            if eng.waited.get(k, 0) >= val:
                continue
            if k not in need or need[k][1] < val:
                need[k] = (sem, val)
        for k, (sem, val) in need.items():
            eng.eng.wait_ge(sem, val)
            eng.waited[k] = val

    def _deps(self, r, w, skip_pe_acc=None):
        toks = []
        for b in r:
            toks.append(b.w)
        for b in w:
            if b is skip_pe_acc:
                continue
            toks.append(b.w)
            toks.extend(b.r)
        return toks

    def op(self, en, fn, r=(), w=(), inc=True, acc=None):
        e = self.E[en]
        toks = self._deps(r, w, skip_pe_acc=acc)
        if acc is not None:
            toks.extend(t for t in acc.r)
            if acc.w is not None and not (acc.w[0] == "eng" and acc.w[1] is e):
                toks.append(acc.w)
        self._wait(e, toks)
        ins = fn(e.eng)
        ent = [ins, None]
        if inc:
            e.count += 1
            ins.then_inc(e.sem, 1)
            ent[1] = e.count
        e.ins.append(ent)
        tok = ("eng", e, len(e.ins) - 1)
        for b in r:
            b.r.append(tok)
        for b in w:
            b.w = tok
            b.r = []
        return tok

    def dma(self, q, out, in_, r=(), w=()):
        e = self.E[q]
        toks = self._deps(r, w)
        slot = self.dsem[q][self.dnext[q] % len(self.dsem[q])]
        self.dnext[q] += 1
        sem, cnt = slot
        if cnt > 0:
            toks.append(("sem", sem, cnt * 16))
        self._wait(e, toks)
        e.eng.dma_start(out=out, in_=in_).then_inc(sem, 16)
        slot[1] = cnt + 1
        tok = ("sem", sem, (cnt + 1) * 16)
        for b in r:
            b.r.append(tok)
        for b in w:
            b.w = tok
            b.r = []
        self.all_dma_tokens.append(tok)
        return tok

    def barrier(self):
        toks = []
        for e in self.E.values():
            if e.ins:
                toks.append(("eng", e, len(e.ins) - 1))
        for q in self.dsem:
            for sem, cnt in self.dsem[q]:
                if cnt > 0:
                    toks.append(("sem", sem, cnt * 16))
        for e in self.E.values():
            self._wait(e, toks)
        for b in self.bufs.values():
            b.w = None
            b.r = []


def build(phases=("all",), debug=()):
    nc = bass.Bass("TRN2", target_bir_lowering=False)
    dt = nc.dram_tensor

    SPECS = dict(xs=[D, NSLOT * SL], cpc=[128, KC], vecs=[128, 8, KC], bada=[128, 8, KC], slotvalid=[128, NSLOT],
                 w_in=[D, PROJ], w_out=[D, D], w_ada=[D, 6 * D], w_adaf=[D, 2 * D], convw=[128, 4, KC], gbias=[128, 8],
                 mlnw=[128, 2048], wr=[D, 36], br=[128, 36], wg=[NEXP, D, DFF], wu=[NEXP, D, DFF], wd=[NEXP, DFF, D],
                 cmat=[128, 5, 128], cmask=[128, 4, SL])
    EXT = {}

    class _Lazy:
        def __init__(self, name):
            self.name = name

        def _ap(self):
            if self.name not in EXT:
                EXT[self.name] = dt(self.name, list(SPECS[self.name]), F32, kind="ExternalInput").ap()
            return EXT[self.name]

        def __getitem__(self, k):
            return self._ap()[k]

        def rearrange(self, *a, **k):
            return self._ap().rearrange(*a, **k)

    xs, cpc, vecs, bada, slotvalid, w_in, w_out, w_ada, w_adaf, convw, gbias, mlnw, wr, br, wg, wu, wd, cmat, cmask = [
        _Lazy(n) for n in ("xs", "cpc", "vecs", "bada", "slotvalid", "w_in", "w_out", "w_ada", "w_adaf", "convw", "gbias",
                           "mlnw", "wr", "br", "wg", "wu", "wd", "cmat", "cmask")]
    outT = dt("outT", [D, 4 * SL], F32, kind="ExternalOutput").ap()

    NT = NSLOT * SL
    qT_scr = dt("qT_scr", [2048, 4 * SL], BF16).ap()
    kT_scr = dt("kT_scr", [2048, NT], BF16).ap()
    v_scr = dt("v_scr", [16, 128, 64, 128], BF16).ap()
    mlk_scr = dt("mlk_scr", [2048, NT + 8], BF16).ap()
    mlq_scr = dt("mlq_scr", [2048, 4 * 520], BF16).ap()
    mlv_scr = dt("mlv_scr", [4, 128, 64, 512], BF16).ap()
    mlo_scr = dt("mlo_scr", [4, 128, 16, 512], BF16).ap()
    if_scr = dt("if_scr", [128, 64, 8], F32).ap()
    yT_scr = dt("yT_scr", [D, 4 * SL], BF16).ap()
    hT_scr = dt("hT_scr", [D, 4 * SL], F32).ap()
    u2T_scr = dt("u2T_scr", [D, 4 * SL], BF16).ap()
    act_scr = dt("act_scr", [NEXP, DFF, 1024], BF16).ap()
    win_bf = dt("win_bf", [128, 56, KC, 256], BF16).ap()
    dbg = {}
    SCR = dict(qT_scr=(qT_scr, BF16), kT_scr=(kT_scr, BF16), v_scr=(v_scr, BF16), mlk_scr=(mlk_scr, BF16), mlq_scr=(mlq_scr, BF16),
               mlv_scr=(mlv_scr, BF16), mlo_scr=(mlo_scr, BF16), if_scr=(if_scr, F32), yT_scr=(yT_scr, BF16), hT_scr=(hT_scr, F32),
               u2T_scr=(u2T_scr, BF16))
    SBD = dict(mod=[128, 8, KC], AB=[128, 6, KC], gwT=[32, 4 * SL])
    for nm in debug:
        if nm in SCR:
            dbg[nm] = dt("dbg_" + nm, list(SCR[nm][0].shape), SCR[nm][1], kind="ExternalOutput").ap()
        else:
            dbg[nm] = dt("dbg_" + nm, SBD[nm], F32, kind="ExternalOutput").ap()

    def on(p):
        return "all" in phases or p in phases

    with ExitStack() as es:
        S = Sched(nc, es)
        B = S.B
        sb = lambda name, shape, dtype=F32: es.enter_context(nc.sbuf_tensor(name, list(shape), dtype))
        cm_f = sb("cm_f", [128, 5, 128])
        cm_b = sb("cm_b", [128, 5, 128], BF16)
        onescol_b = sb("onescol_b", [128, 1], BF16)
        mod = sb("mod", [128, 8, KC])
        vec = sb("vec", [128, 8, KC])
        AB = sb("AB", [128, 6, KC])
        sval = sb("sval", [128, NSLOT])
        gwT = sb("gwT", [32, 4 * SL])
        IDF, UTm, LTm, LEm, ONE = 0, 1, 2, 3, 4

        S.dma("sp", cm_f[:], cmat[:], w=[B("cm_f")])
        S.dma("pool", cm_b[:], cmat[:], w=[B("cm_b")])
        S.dma("sp", vec[:], vecs[:], w=[B("vec")])
        S.dma("sp", sval[:], slotvalid[:], w=[B("sval")])
        S.op("dve", lambda e: e.memset(onescol_b[:], 1.0), w=[B("onescol")])

        if on("p0"):
            with ExitStack() as ps_:
                sbp = lambda name, shape, dtype=F32: ps_.enter_context(nc.sbuf_tensor(name, list(shape), dtype))
                PS = [ps_.enter_context(nc.psum_tensor("p0ps%d" % i, [128, 512], F32)) for i in range(7)]
                PSB = ps_.enter_context(nc.psum_tensor("p0psb", [128, 512], BF16))
                cf = sbp("cf", [128, KC])
                cb = sbp("cb", [128, KC], BF16)
                bad = sbp("bad", [128, 8, KC])
                wa = [sbp("wa%d" % i, [128, KC, 512], BF16) for i in range(2)]
                S.dma("sp", cf[:], cpc[:], w=[B("cf")])
                S.dma("sp", bad[:], bada[:], w=[B("bad")])
                S.op("act", lambda e: e.activation(out=cb[:], in_=cf[:], func=AF.Silu), r=[B("cf")], w=[B("cb")])
                gi = 0
                for v in range(8):
                    src = w_ada if v < 6 else w_adaf
                    c0 = (v if v < 6 else v - 6) * D
                    pb = B("ps", v % 2)
                    pt = PS[v % 2]
                    for g in range(8):
                        wt = wa[gi % 2]
                        wb_ = B("wa", gi % 2)
                        gi += 1
                        S.dma("pool", wt[:], src[:, c0 + g * 512: c0 + (g + 1) * 512].rearrange("(c p) n -> p c n", p=128), w=[wb_])
                        for cc in range(4):
                            col = g * 4 + cc
                            for kc in range(KC):
                                S.op("pe", lambda e, wt=wt, cc=cc, kc=kc, col=col, pt=pt: e.matmul(
                                    pt[:, col:col + 1], lhsT=wt[:, kc, cc * 128:(cc + 1) * 128], rhs=cb[:, kc:kc + 1],
                                    start=(kc == 0), stop=(kc == KC - 1)),
                                    r=[wb_, B("cb")], acc=pb, w=[pb], inc=(kc == KC - 1))
                    S.op("dve", lambda e, v=v, pt=pt: e.tensor_tensor(out=mod[:, v, :], in0=pt[:, 0:KC], in1=bad[:, v, :], op=ALU.add),
                         r=[pb, B("bad")], w=[B("mod")])
                for i, (nwi, sci, shi) in enumerate(((0, 1, 0), (1, 4, 3), (2, 7, 6))):
                    S.op("dve", lambda e, i=i, nwi=nwi, sci=sci: e.scalar_tensor_tensor(
                        out=AB[:, 2 * i, :], in0=mod[:, sci, :], scalar=1.0, in1=vec[:, nwi, :], op0=ALU.add, op1=ALU.mult),
                        r=[B("mod"), B("vec")], w=[B("AB")])
                    S.op("dve", lambda e, i=i, shi=shi: e.tensor_copy(out=AB[:, 2 * i + 1, :], in_=mod[:, shi, :]),
                         r=[B("mod")], w=[B("AB")])
            S.barrier()
        if "mod" in dbg:
            S.dma("sp", dbg["mod"], mod[:], r=[B("mod")])
            S.dma("sp", dbg["AB"], AB[:], r=[B("AB")])

        O_SBQ, O_SBK, O_SBV, O_MLQ, O_MLK, O_MLV, O_MLO, O_IF = 0, 2048, 4096, 6144, 8192, 10240, 12288, 14336
        if on("p1"):
            with ExitStack() as ps_:
                sbp = lambda name, shape, dtype=F32: ps_.enter_context(nc.sbuf_tensor(name, list(shape), dtype))
                PS = [ps_.enter_context(nc.psum_tensor("p1ps%d" % i, [128, 512], F32)) for i in range(7)]
                PSB = ps_.enter_context(nc.psum_tensor("p1psb", [128, 512], BF16))
                xh = [sbp("xh%d" % i, [128, 16, SL]) for i in range(2)]
                uT = sbp("uT", [128, KC, 2 * SL], BF16)
                W = [sbp("W%d" % i, [128, KC, 256], BF16) for i in range(3)]
                Wif = sbp("Wif", [128, KC, 8], BF16)
                sq = [sbp("sq%d" % i, [128, SL], BF16) for i in range(3)]
                tmp = [sbp("tmp%d" % i, [128, SL]) for i in range(3)]
                rstd = sbp("rstd", [128, SL])
                ABs = sbp("ABs", [128, 2, KC])
                ev = [sbp("ev%d" % i, [128, SL], BF16) for i in range(4)]
                evf = [sbp("evf%d" % i, [128, 8]) for i in range(2)]
                zt = sbp("zt", [128, 8], BF16)
                S.op("dve", lambda e: e.memset(zt[:], 0.0), w=[B("zt")])
                for c in range(16):
                    S.dma("sp", mlk_scr[c * 128:(c + 1) * 128, 0:8], zt[:], r=[B("zt")], w=[B("mlk_scr", "halo")])
                S.dma("pool", Wif[:], w_in[:, O_IF:O_IF + 8].rearrange("(c p) n -> p c n", p=128), w=[B("Wif")])
                wi = [0]
                evi = [0]
                psi = [0]

                seenW = set()

                def loadW(c0):
                    k = wi[0] % 3
                    wi[0] += 1
                    gidx = c0 // 256
                    if c0 not in seenW:
                        seenW.add(c0)
                        S.dma("pool", W[k][:], w_in[:, c0:c0 + 256].rearrange("(c p) n -> p c n", p=128), w=[B("W", k)])
                        S.dma("act", win_bf[:, gidx, :, :], W[k][:], r=[B("W", k)], w=[B("win_bf", gidx)])
                    else:
                        S.dma("sp" if (wi[0] % 2) else "act", W[k][:], win_bf[:, gidx, :, :], r=[B("win_bf", gidx)], w=[B("W", k)])
                    return W[k], B("W", k)

                def nextps():
                    k = psi[0] % 6
                    psi[0] += 1
                    return PS[k], B("ps", k)

                def evac(pt, pb, n, dst, dstbufs, use_act):
                    k = evi[0] % 4
                    evi[0] += 1
                    et, eb = ev[k], B("ev", k)
                    if use_act:
                        S.op("act", lambda e: e.activation(out=et[:, 0:n], in_=pt[:, 0:n], func=AF.Copy), r=[pb], w=[eb])
                    else:
                        S.op("dve", lambda e: e.tensor_copy(out=et[:, 0:n], in_=pt[:, 0:n]), r=[pb], w=[eb])
                    S.dma("sp", dst, et[:, 0:n], r=[eb], w=dstbufs)

                def feat_major(c0, dst_rows, tok_lo, ntok, dst, dstbufs):
                    Wt, Wb = loadW(c0)
                    for cc in range(2):
                        pt, pb = nextps()
                        for kc in range(KC):
                            S.op("pe", lambda e, kc=kc, cc=cc, pt=pt, Wt=Wt: e.matmul(
                                pt[:, 0:ntok], lhsT=Wt[:, kc, cc * 128:(cc + 1) * 128], rhs=uT[:, kc, tok_lo:tok_lo + ntok],
                                start=(kc == 0), stop=(kc == KC - 1)),
                                r=[Wb, B("uT")], acc=pb, w=[pb], inc=(kc == KC - 1))
                        evac(pt, pb, ntok, dst(cc), dstbufs, use_act=(cc == 0))

                def tok_major(c0, tblocks, dst, dstbufs):
                    Wt, Wb = loadW(c0)
                    for i, tb in enumerate(tblocks):
                        pt, pb = nextps()
                        for kc in range(KC):
                            S.op("pe", lambda e, kc=kc, tb=tb, pt=pt, Wt=Wt: e.matmul(
                                pt[:, 0:256], lhsT=uT[:, kc, tb * 128:(tb + 1) * 128], rhs=Wt[:, kc, :],
                                start=(kc == 0), stop=(kc == KC - 1)),
                                r=[Wb, B("uT")], acc=pb, w=[pb], inc=(kc == KC - 1))
                        evac(pt, pb, 256, dst(tb), dstbufs, use_act=(i % 2 == 0))

                for T in range(NSLOT // 2):
                    for half in range(2):
                        s = 2 * T + half
                        tcol = half * SL
                        S.op("dve", lambda e, s=s: e.tensor_scalar(out=ABs[:, 0, :], in0=AB[:, 0, :], scalar1=sval[:, s:s + 1], scalar2=None, op0=ALU.mult),
                             r=[B("AB"), B("sval")], w=[B("ABs")])
                        S.op("dve", lambda e, s=s: e.tensor_scalar(out=ABs[:, 1, :], in0=AB[:, 1, :], scalar1=sval[:, s:s + 1], scalar2=None, op0=ALU.mult),
                             r=[B("AB"), B("sval")], w=[B("ABs")])
                        pt, pb = PS[6], B("ps", 6)
                        for pas in range(2):
                            for hh in range(2):
                                xt, xb = xh[hh], B("xh", hh)
                                S.dma("act" if hh else "sp", xt[:], xs[hh * 2048:(hh + 1) * 2048, s * SL:(s + 1) * SL].rearrange("(c p) t -> p c t", p=128), w=[xb])
                                for k16 in range(16):
                                    kc = hh * 16 + k16
                                    if pas == 0:
                                        qi = kc % 3
                                        S.op("act", lambda e, qi=qi, xt=xt, k16=k16: e.activation(out=sq[qi][:], in_=xt[:, k16, :], func=AF.Square),
                                             r=[xb], w=[B("sq", qi)])
                                        S.op("pe", lambda e, qi=qi, kc=kc: e.matmul(pt[:], lhsT=cm_b[:, ONE, :], rhs=sq[qi][:], start=(kc == 0), stop=(kc == KC - 1)),
                                             r=[B("sq", qi), B("cm_b")], acc=pb, w=[pb], inc=True)
                                    else:
                                        ti = kc % 3
                                        S.op("dve", lambda e, ti=ti, xt=xt, k16=k16: e.tensor_tensor(out=tmp[ti][:], in0=xt[:, k16, :], in1=rstd[:], op=ALU.mult),
                                             r=[xb, B("rstd")], w=[B("tmp", ti)])
                                        S.op("act", lambda e, ti=ti, kc=kc, tcol=tcol: e.activation(
                                            out=uT[:, kc, tcol:tcol + SL], in_=tmp[ti][:], func=AF.Identity,
                                            scale=ABs[:, 0, kc:kc + 1], bias=ABs[:, 1, kc:kc + 1]),
                                            r=[B("tmp", ti), B("ABs")], w=[B("uT")])
                            if pas == 0:
                                S.op("dve", lambda e: e.tensor_scalar(out=rstd[:], in0=pt[:], scalar1=1.0 / D, scalar2=EPS, op0=ALU.mult, op1=ALU.add),
                                     r=[pb], w=[B("rstd")])
                                S.op("act", lambda e: e.activation(out=rstd[:], in_=rstd[:], func=AF.Sqrt), r=[B("rstd")], w=[B("rstd")])
                                S.op("dve", lambda e: e.reciprocal(out=rstd[:], in_=rstd[:]), r=[B("rstd")], w=[B("rstd")])
                    t0 = 2 * T * SL
                    for (obase, scr, off, scrn) in ((O_SBK, kT_scr, 0, 'kT_scr'), (O_MLK, mlk_scr, 8, 'mlk_scr')):
                        for g in range(8):
                            Wt, Wb = loadW(obase + g * 256)
                            for half in range(2):
                                for cc in range(2):
                                    pt, pb = nextps()
                                    for kc in range(KC):
                                        S.op("pe", lambda e, kc=kc, cc=cc, pt=pt, Wt=Wt, half=half: e.matmul(
                                            pt[:], lhsT=Wt[:, kc, cc * 128:(cc + 1) * 128], rhs=uT[:, kc, half * SL:(half + 1) * SL],
                                            start=(kc == 0), stop=(kc == KC - 1)),
                                            r=[Wb, B("uT")], acc=pb, w=[pb], inc=(kc == KC - 1))
                                    r0 = g * 256 + cc * 128
                                    evac(pt, pb, SL, scr[r0:r0 + 128, off + t0 + half * SL: off + t0 + (half + 1) * SL], [B(scrn)], use_act=(cc == 0))
                    for g in range(8):
                        Wt, Wb = loadW(O_SBV + g * 256)
                        for tb in range(8):
                            pt, pb = nextps()
                            for kc in range(KC):
                                S.op("pe", lambda e, kc=kc, tb=tb, pt=pt, Wt=Wt: e.matmul(
                                    pt[:, 0:256], lhsT=uT[:, kc, tb * 128:(tb + 1) * 128], rhs=Wt[:, kc, :],
                                    start=(kc == 0), stop=(kc == KC - 1)),
                                    r=[Wb, B("uT")], acc=pb, w=[pb], inc=(kc == KC - 1))
                            blk = 2 * T * 4 + tb
                            k = evi[0] % 4
                            evi[0] += 1
                            et, eb = ev[k], B("ev", k)
                            if tb % 2:
                                S.op("act", lambda e, et=et, pt=pt: e.activation(out=et[:, 0:256], in_=pt[:, 0:256], func=AF.Copy), r=[pb], w=[eb])
                            else:
                                S.op("dve", lambda e, et=et, pt=pt: e.tensor_copy(out=et[:, 0:256], in_=pt[:, 0:256]), r=[pb], w=[eb])
                            for hh in range(2):
                                S.dma("sp", v_scr[2 * g + hh, :, blk, :], et[:, hh * 128:(hh + 1) * 128], r=[eb], w=[B("v_scr")])
                    for g in range(8):
                        Wt, Wb = loadW(O_MLV + g * 256)
                        for tb in range(8):
                            pt, pb = nextps()
                            for kc in range(KC):
                                S.op("pe", lambda e, kc=kc, tb=tb, pt=pt, Wt=Wt: e.matmul(
                                    pt[:, 0:256], lhsT=uT[:, kc, tb * 128:(tb + 1) * 128], rhs=Wt[:, kc, :],
                                    start=(kc == 0), stop=(kc == KC - 1)),
                                    r=[Wb, B("uT")], acc=pb, w=[pb], inc=(kc == KC - 1))
                            blk = 2 * T * 4 + tb
                            evac(pt, pb, 256, mlv_scr[g // 2, :, blk, (g % 2) * 256:(g % 2 + 1) * 256], [B("mlv_scr")], use_act=(tb % 2 == 1))
                    for tb in range(8):
                        pt, pb = nextps()
                        for kc in range(KC):
                            S.op("pe", lambda e, kc=kc, tb=tb, pt=pt: e.matmul(
                                pt[:, 0:8], lhsT=uT[:, kc, tb * 128:(tb + 1) * 128], rhs=Wif[:, kc, :],
                                start=(kc == 0), stop=(kc == KC - 1)),
                                r=[B("Wif"), B("uT")], acc=pb, w=[pb], inc=(kc == KC - 1))
                        k = tb % 2
                        S.op("dve", lambda e, k=k, pt=pt: e.tensor_copy(out=evf[k][:], in_=pt[:, 0:8]), r=[pb], w=[B("evf", k)])
                        S.dma("sp", if_scr[:, 2 * T * 4 + tb, :], evf[k][:], r=[B("evf", k)], w=[B("if_scr")])
                    if (2 * T + 1) in OWN:
                        oi = OWN.index(2 * T + 1)
                        for g in range(8):
                            Wt, Wb = loadW(O_SBQ + g * 256)
                            for cc in range(2):
                                pt, pb = nextps()
                                for kc in range(KC):
                                    S.op("pe", lambda e, kc=kc, cc=cc, pt=pt, Wt=Wt: e.matmul(
                                        pt[:], lhsT=Wt[:, kc, cc * 128:(cc + 1) * 128], rhs=uT[:, kc, SL:2 * SL],
                                        start=(kc == 0), stop=(kc == KC - 1)),
                                        r=[Wb, B("uT")], acc=pb, w=[pb], inc=(kc == KC - 1))
                                r0 = g * 256 + cc * 128
                                evac(pt, pb, SL, qT_scr[r0:r0 + 128, oi * SL:(oi + 1) * SL], [B("qT_scr")], use_act=(cc == 0))
                        for g in range(8):
                            Wt, Wb = loadW(O_MLQ + g * 256)
                            for cc in range(2):
                                r0 = g * 256 + cc * 128
                                for (lo, n, dcol) in ((SL, SL, 8), (SL - 8, 8, 0)):
                                    pt, pb = nextps()
                                    for kc in range(KC):
                                        S.op("pe", lambda e, kc=kc, cc=cc, pt=pt, Wt=Wt, lo=lo, n=n: e.matmul(
                                            pt[:, 0:n], lhsT=Wt[:, kc, cc * 128:(cc + 1) * 128], rhs=uT[:, kc, lo:lo + n],
                                            start=(kc == 0), stop=(kc == KC - 1)),
                                            r=[Wb, B("uT")], acc=pb, w=[pb], inc=(kc == KC - 1))
                                    evac(pt, pb, n, mlq_scr[r0:r0 + 128, oi * 520 + dcol: oi * 520 + dcol + n], [B("mlq_scr")], use_act=(cc == 0))
                        for g in range(8):
                            Wt, Wb = loadW(O_MLO + g * 256)
                            for tb in range(4, 8):
                                pt, pb = nextps()
                                for kc in range(KC):
                                    S.op("pe", lambda e, kc=kc, tb=tb, pt=pt, Wt=Wt: e.matmul(
                                        pt[:, 0:256], lhsT=uT[:, kc, tb * 128:(tb + 1) * 128], rhs=Wt[:, kc, :],
                                        start=(kc == 0), stop=(kc == KC - 1)),
                                        r=[Wb, B("uT")], acc=pb, w=[pb], inc=(kc == KC - 1))
                                evac(pt, pb, 256, mlo_scr[g // 2, :, oi * 4 + tb - 4, (g % 2) * 256:(g % 2 + 1) * 256], [B("mlo_scr")], use_act=(tb % 2 == 1))
            S.barrier()
        if on("p2"):
            with ExitStack() as ps_:
                sbp = lambda name, shape, dtype=F32: ps_.enter_context(nc.sbuf_tensor(name, list(shape), dtype))
                PS = [ps_.enter_context(nc.psum_tensor("p2ps%d" % i, [128, 512], F32)) for i in range(8)]
                kTt = [sbp("kTt%d" % i, [128, NT], BF16) for i in range(2)]
                vt = [sbp("vt%d" % i, [128, 64, 128], BF16) for i in range(2)]
                qt = [sbp("qt%d" % i, [128, 4 * SL], BF16) for i in range(2)]
                msk = sbp("msk", [128, 4, SL])
                e_t = [[sbp("e_t%d_%d" % (s_, i), [128, SL]) for i in range(2)] for s_ in range(2)]
                a_t = [[sbp("a_t%d_%d" % (s_, i), [128, SL]) for i in range(2)] for s_ in range(2)]
                spf = [sbp("spf%d" % s_, [128, SL]) for s_ in range(2)]
                spb = [[sbp("spb%d_%d" % (s_, i), [128, SL], BF16) for i in range(2)] for s_ in range(2)]
                E_t = [[sbp("E_t%d_%d" % (s_, i), [128, SL]) for i in range(2)] for s_ in range(2)]
                wf = [sbp("wf%d" % s_, [128, SL]) for s_ in range(2)]
                wb = [[sbp("wb%d_%d" % (s_, i), [128, SL], BF16) for i in range(2)] for s_ in range(2)]
                ob = [sbp("ob%d" % s_, [128, SL], BF16) for s_ in range(2)]
                Rc = [sbp("Rc%d" % s_, [128, SL]) for s_ in range(2)]
                S.dma("sp", msk[:], cmask[:], w=[B("msk")])
                if not on("p1") and not _os.environ.get("P2NOINIT"):
                    S.op("dve", lambda e: e.memset(kTt[0][:], 0.01), w=[B("kTt", 0)])
                    for h_ in range(16):
                        S.dma("sp", kT_scr[h_ * 128:(h_ + 1) * 128, :], kTt[0][:], r=[B("kTt", 0)], w=[B("kT_scr")])
                        S.dma("sp", qT_scr[h_ * 128:(h_ + 1) * 128, :], kTt[0][:, 0:2048], r=[B("kTt", 0)], w=[B("qT_scr")])
                        S.dma("sp", v_scr[h_], kTt[0][:].rearrange("p (a b) -> p a b", b=128), r=[B("kTt", 0)], w=[B("v_scr")])
                scale = float(1.0 / np.sqrt(128.0))
                nh = int(_os.environ.get('P2H', '16'))
                for hp in range(nh // 2):
                    for s_ in range(2):
                        h = 2 * hp + s_
                        S.dma("sp", kTt[s_][:], kT_scr[h * 128:(h + 1) * 128, :], r=[B("kT_scr")], w=[B("kTt", s_)])
                        S.dma("act", vt[s_][:], v_scr[h], r=[B("v_scr")], w=[B("vt", s_)])
                        S.dma("sp", qt[s_][:], qT_scr[h * 128:(h + 1) * 128, :], r=[B("qT_scr")], w=[B("qt", s_)])
                    for oi, so in list(enumerate(OWN))[:int(_os.environ.get('P2O', '4'))]:
                        nblk = (so + 1) * 4

                        def stage1(s_, bi):
                            jb = nblk - 1 - bi
                            m = jb - so * 4
                            i2 = bi % 2
                            zp, zb = PS[4 * s_], B("ps", 4 * s_)
                            S.op("pe", lambda e: e.matmul(zp[:], lhsT=kTt[s_][:, jb * 128:(jb + 1) * 128], rhs=qt[s_][:, oi * SL:(oi + 1) * SL], start=True, stop=True),
                                 r=[B("kTt", s_), B("qt", s_)], w=[zb])
                            S.op("act", lambda e: e.activation(out=e_t[s_][i2][:], in_=zp[:], func=AF.Exp, scale=scale),
                                 r=[zb], w=[B("e_t", s_, i2), B("zser", s_)])
                            if bi == 0:
                                S.op("dve", lambda e: e.tensor_scalar(out=a_t[s_][i2][:], in0=zp[:], scalar1=scale, scalar2=None, op0=ALU.mult),
                                     r=[zb, B("zser", s_)], w=[B("a_t", s_, i2)])
                            else:
                                S.op("dve", lambda e: e.scalar_tensor_tensor(out=a_t[s_][i2][:], in0=zp[:], scalar=scale, in1=Rc[s_][:], op0=ALU.mult, op1=ALU.subtract),
                                     r=[zb, B("Rc", s_), B("zser", s_)], w=[B("a_t", s_, i2)])
                            if m >= 0:
                                S.op("act", lambda e: e.activation(out=spf[s_][:], in_=e_t[s_][i2][:], func=AF.Ln, bias=1.0),
                                     r=[B("e_t", s_, i2)], w=[B("spf", s_)])
                                S.op("pool", lambda e: e.tensor_tensor(out=spb[s_][i2][:], in0=spf[s_][:], in1=msk[:, m, :], op=ALU.mult),
                                     r=[B("spf", s_), B("msk")], w=[B("spb", s_, i2)])
                            else:
                                S.op("act", lambda e: e.activation(out=spb[s_][i2][:], in_=e_t[s_][i2][:], func=AF.Ln, bias=1.0),
                                     r=[B("e_t", s_, i2)], w=[B("spb", s_, i2)])

                        def stage2(s_, bi):
                            jb = nblk - 1 - bi
                            m = jb - so * 4
                            i2 = bi % 2
                            xp, xb = PS[4 * s_ + 1], B("ps", 4 * s_ + 1)
                            yp, yb = PS[4 * s_ + 2], B("ps", 4 * s_ + 2)
                            S.op("pe", lambda e: e.matmul(xp[:], lhsT=cm_b[:, UTm, :], rhs=spb[s_][i2][:], start=True, stop=True),
                                 r=[B("spb", s_, i2), B("cm_b")], w=[xb])
                            if bi < nblk - 1:
                                S.op("pe", lambda e: e.matmul(yp[:], lhsT=cm_b[:, ONE, :], rhs=spb[s_][i2][:], start=True, stop=True),
                                     r=[B("spb", s_, i2), B("cm_b")], w=[yb])
                            S.op("dve", lambda e: e.tensor_tensor(out=E_t[s_][i2][:], in0=a_t[s_][i2][:], in1=xp[:], op=ALU.subtract),
                                 r=[B("a_t", s_, i2), xb], w=[B("E_t", s_, i2)])
                            if bi < nblk - 1:
                                if bi == 0:
                                    S.op("dve", lambda e: e.tensor_copy(out=Rc[s_][:], in_=yp[:]), r=[yb], w=[B("Rc", s_)])
                                else:
                                    S.op("dve", lambda e: e.tensor_tensor(out=Rc[s_][:], in0=Rc[s_][:], in1=yp[:], op=ALU.add), r=[yb, B("Rc", s_)], w=[B("Rc", s_)])
                            if m >= 0:
                                S.op("act", lambda e: e.activation(out=wf[s_][:], in_=E_t[s_][i2][:], func=AF.Exp),
                                     r=[B("E_t", s_, i2)], w=[B("wf", s_)])
                                S.op("pool", lambda e: e.tensor_tensor(out=wb[s_][i2][:], in0=wf[s_][:], in1=msk[:, m, :], op=ALU.mult),
                                     r=[B("wf", s_), B("msk")], w=[B("wb", s_, i2)])
                            else:
                                S.op("act", lambda e: e.activation(out=wb[s_][i2][:], in_=E_t[s_][i2][:], func=AF.Exp),
                                     r=[B("E_t", s_, i2)], w=[B("wb", s_, i2)])

                        def stage3(s_, bi):
                            jb = nblk - 1 - bi
                            i2 = bi % 2
                            outp, outb = PS[4 * s_ + 3], B("ps", 4 * s_ + 3)
                            S.op("pe", lambda e: e.matmul(outp[:], lhsT=vt[s_][:, jb, :], rhs=wb[s_][i2][:], start=(bi == 0), stop=(bi == nblk - 1)),
                                 r=[B("wb", s_, i2), B("vt", s_)], acc=outb, w=[outb], inc=True)

                        for bi in range(nblk + 1):
                            if bi < nblk:
                                stage1(0, bi)
                                stage1(1, bi)
                            if bi >= 1:
                                stage3(0, bi - 1)
                                stage3(1, bi - 1)
                            if bi < nblk:
                                stage2(0, bi)
                                stage2(1, bi)
                        for s_ in range(2):
                            h = 2 * hp + s_
                            outp, outb = PS[4 * s_ + 3], B("ps", 4 * s_ + 3)
                            S.op("act", lambda e, s_=s_, outp=outp: e.activation(out=ob[s_][:], in_=outp[:], func=AF.Copy), r=[outb], w=[B("ob", s_)])
                            S.dma("sp", yT_scr[h * 128:(h + 1) * 128, oi * SL:(oi + 1) * SL], ob[s_][:], r=[B("ob", s_)], w=[B("yT_scr")])
            S.barrier()
        if on("p3"):
            with ExitStack() as ps_:
                sbp = lambda name, shape, dtype=F32: ps_.enter_context(nc.sbuf_tensor(name, list(shape), dtype))
                PS = [ps_.enter_context(nc.psum_tensor("p3ps%d" % i, [128, 512], F32)) for i in range(7)]
                PSB = ps_.enter_context(nc.psum_tensor("p3psb", [128, 512], BF16))
                cw = sbp("cw", [128, 4, KC])
                gb = sbp("gb", [128, 8])
                nwb = sbp("nwb", [128, 2048])
                iff = sbp("iff", [128, 64, 8])
                lf = sbp("lf", [128, 64])
                ii = sbp("ii", [128, 64])
                bcum = sbp("bcum", [128, 64])
                bL = sbp("bL", [128, 64])
                gA = sbp("gA", [128, 64])
                gK = sbp("gK", [128, 64])
                dec = sbp("dec", [128, 64])
                ebi = sbp("ebi", [128, 64])
                t64 = sbp("t64", [128, 64])
                kpre = sbp("kpre", [128, 4, SL + 8], BF16)
                kcv = sbp("kcv", [128, 4, SL])
                kTc = sbp("kTc", [128, 4, SL], BF16)
                qpre = sbp("qpre", [128, 4, SL + 8], BF16)
                qTc = sbp("qTc", [128, 4, SL], BF16)
                vch = sbp("vch", [128, 4, 512], BF16)
                och = sbp("och", [128, 4, 512], BF16)
                kw = [sbp("kw%d" % i, [128, 512], BF16) for i in range(2)]
                Cst = sbp("Cst", [128, 4, 512])
                Cbf = sbp("Cbf", [128, 4, 512], BF16)
                nst = sbp("nst", [128, 4])
                nbf = sbp("nbf", [128, 4], BF16)
                Ab = sbp("Ab", [128, 128], BF16)
                hm = sbp("hm", [128, 512])
                sg = sbp("sg", [128, 512])
                y1 = sbp("y1", [128, 512])
                y2 = sbp("y2", [128, 512], BF16)
                yTb = sbp("yTb", [128, 512], BF16)
                sm = sbp("sm", [128, 8])
                S.dma("sp", cw[:], convw[:], w=[B("cw")])
                S.dma("sp", gb[:], gbias[:], w=[B("gb")])
                S.dma("sp", nwb[:], mlnw[:], w=[B("nwb")])
                S.dma("sp", iff[:], if_scr, r=[B("if_scr")], w=[B("iff")])
                isq = 1.0 / np.sqrt(512.0)

                def conv_silu(pre, preb, out_bf, outb, chbase, hd):
                    for dc in range(4):
                        ch = chbase + hd * 4 + dc
                        S.op("dve", lambda e, dc=dc, ch=ch: e.tensor_scalar(out=kcv[:, dc, :], in0=pre[:, dc, 5:5 + SL], scalar1=cw[:, 0, ch:ch + 1], scalar2=None, op0=ALU.mult),
                             r=[preb, B("cw")], w=[B("kcv")])
                        for i in range(1, 4):
                            S.op("dve", lambda e, dc=dc, ch=ch, i=i: e.scalar_tensor_tensor(
                                out=kcv[:, dc, :], in0=pre[:, dc, 5 + i:5 + i + SL], scalar=cw[:, i, ch:ch + 1], in1=kcv[:, dc, :], op0=ALU.mult, op1=ALU.add),
                                r=[preb, B("cw"), B("kcv")], w=[B("kcv")])
                        S.op("act", lambda e, dc=dc: e.activation(out=out_bf[:, dc, :], in_=kcv[:, dc, :], func=AF.Silu), r=[B("kcv")], w=[outb])

                for hd in range(4):
                    S.op("dve", lambda e, hd=hd: e.tensor_scalar(out=ii[:], in0=iff[:, :, hd], scalar1=gb[:, hd:hd + 1], scalar2=None, op0=ALU.add),
                         r=[B("iff"), B("gb")], w=[B("ii")])
                    S.op("dve", lambda e, hd=hd: e.tensor_scalar(out=t64[:], in0=iff[:, :, 4 + hd], scalar1=gb[:, 4 + hd:5 + hd], scalar2=None, op0=ALU.add),
                         r=[B("iff"), B("gb")], w=[B("t64")])
                    S.op("act", lambda e: e.activation(out=t64[:], in_=t64[:], func=AF.Exp, scale=-1.0), r=[B("t64")], w=[B("t64")])
                    S.op("act", lambda e: e.activation(out=t64[:], in_=t64[:], func=AF.Ln, bias=1.0), r=[B("t64")], w=[B("t64")])
                    S.op("dve", lambda e: e.tensor_scalar(out=lf[:], in0=t64[:], scalar1=-1.0, scalar2=None, op0=ALU.mult), r=[B("t64")], w=[B("lf")])
                    p0, p0b = PS[0], B("ps", 0)
                    S.op("pe", lambda e: e.matmul(p0[:, 0:64], lhsT=cm_f[:, LEm, :], rhs=lf[:], start=True, stop=True), r=[B("cm_f"), B("lf")], w=[p0b])
                    S.op("pe", lambda e: e.matmul(p0[:, 64:128], lhsT=cm_f[:, ONE, :], rhs=lf[:], start=True, stop=True), r=[B("cm_f"), B("lf")], w=[p0b])
                    S.op("dve", lambda e: e.tensor_copy(out=bcum[:], in_=p0[:, 0:64]), r=[p0b], w=[B("bcum")])
                    S.op("dve", lambda e: e.tensor_copy(out=bL[:], in_=p0[:, 64:128]), r=[p0b], w=[B("bL")])
                    S.op("act", lambda e: e.activation(out=dec[:], in_=bL[:], func=AF.Exp), r=[B("bL")], w=[B("dec")])
                    S.op("act", lambda e: e.activation(out=ebi[:], in_=bcum[:], func=AF.Exp, scale=-1.0), r=[B("bcum")], w=[B("ebi")])
                    S.op("dve", lambda e: e.tensor_tensor(out=t64[:], in0=ii[:], in1=bcum[:], op=ALU.subtract), r=[B("ii"), B("bcum"), B("lf")], w=[B("t64")])
                    S.op("act", lambda e: e.activation(out=gA[:], in_=t64[:], func=AF.Exp), r=[B("t64")], w=[B("gA")])
                    S.op("dve", lambda e: e.tensor_scalar(out=gA[:], in0=gA[:], scalar1=float(isq), scalar2=None, op0=ALU.mult), r=[B("gA")], w=[B("gA")])
                    S.op("dve", lambda e: e.tensor_tensor(out=gK[:], in0=gA[:], in1=dec[:], op=ALU.mult), r=[B("gA"), B("dec")], w=[B("gK")])
                    S.op("dve", lambda e: e.memset(Cst[:], 0.0), w=[B("Cst", d_) for d_ in range(4)])
                    S.op("pool", lambda e: e.memset(Cbf[:], 0.0), w=[B("Cbf")])
                    S.op("dve", lambda e: e.memset(nst[:], 0.0), w=[B("nst")])
                    S.op("pool", lambda e: e.memset(nbf[:], 0.0), w=[B("nbf")])
                    for s in range(NSLOT):
                        own = s in OWN
                        S.dma("sp", kpre[:], mlk_scr[hd * 512:(hd + 1) * 512, s * SL: s * SL + SL + 8].rearrange("(c p) t -> p c t", p=128),
                              r=[B("mlk_scr"), B("mlk_scr", "halo")], w=[B("kpre")])
                        conv_silu(kpre, B("kpre"), kTc, B("kTc"), 16, hd)
                        if own:
                            oi = OWN.index(s)
                            S.dma("act", qpre[:], mlq_scr[hd * 512:(hd + 1) * 512, oi * 520: oi * 520 + 520].rearrange("(c p) t -> p c t", p=128),
                                  r=[B("mlq_scr")], w=[B("qpre")])
                            conv_silu(qpre, B("qpre"), qTc, B("qTc"), 0, hd)
                            S.dma("act", och[:], mlo_scr[hd, :, oi * 4:(oi + 1) * 4, :], r=[B("mlo_scr")], w=[B("och")])
                        S.dma("sp", vch[:], mlv_scr[hd, :, s * 4:(s + 1) * 4, :], r=[B("mlv_scr")], w=[B("vch")])
                        for c4 in range(4):
                            c = s * 4 + c4
                            tsl = slice(c4 * 128, (c4 + 1) * 128)
                            for dc in range(4):
                                S.op("pe", lambda e, dc=dc, tsl=tsl: e.transpose(PSB[:, dc * 128:(dc + 1) * 128], kTc[:, dc, tsl], cm_b[:, IDF, :]),
                                     r=[B("kTc"), B("cm_b")], w=[B("psb")])
                            kwi = c % 2
                            S.op("dve", lambda e, kwi=kwi, c=c: e.tensor_scalar(out=kw[kwi][:], in0=PSB[:], scalar1=gK[:, c:c + 1], scalar2=None, op0=ALU.mult),
                                 r=[B("psb"), B("gK")], w=[B("kw", kwi)])
                            if own:
                                p1, p1b = PS[1], B("ps", 1)
                                for dc in range(4):
                                    S.op("pe", lambda e, dc=dc, tsl=tsl: e.matmul(p1[:, 0:128], lhsT=kTc[:, dc, tsl], rhs=qTc[:, dc, tsl], start=(dc == 0), stop=(dc == 3)),
                                         r=[B("kTc"), B("qTc")], acc=p1b, w=[p1b], inc=(dc == 3))
                                S.op("dve", lambda e, c=c: e.scalar_tensor_tensor(out=Ab[:], in0=p1[:, 0:128], scalar=gA[:, c:c + 1], in1=cm_f[:, LEm, :], op0=ALU.mult, op1=ALU.mult),
                                     r=[p1b, B("gA"), B("cm_f")], w=[B("Ab")])
                                p2, p2b = PS[2], B("ps", 2)
                                for dc in range(4):
                                    S.op("pe", lambda e, dc=dc, tsl=tsl: e.matmul(p2[:], lhsT=qTc[:, dc, tsl], rhs=Cbf[:, dc, :], start=(dc == 0), stop=False),
                                         r=[B("qTc"), B("Cbf")], acc=p2b, w=[p2b], inc=False)
                                S.op("pe", lambda e, c4=c4: e.matmul(p2[:], lhsT=Ab[:], rhs=vch[:, c4, :], start=False, stop=True),
                                     r=[B("Ab"), B("vch")], acc=p2b, w=[p2b])
                                p3, p3b = PS[3], B("ps", 3)
                                for dc in range(4):
                                    S.op("pe", lambda e, dc=dc, tsl=tsl: e.matmul(p3[:, 0:1], lhsT=qTc[:, dc, tsl], rhs=nbf[:, dc:dc + 1], start=(dc == 0), stop=False),
                                         r=[B("qTc"), B("nbf")], acc=p3b, w=[p3b], inc=False)
                                S.op("pe", lambda e: e.matmul(p3[:, 0:1], lhsT=Ab[:], rhs=onescol_b[:], start=False, stop=True),
                                     r=[B("Ab"), B("onescol")], acc=p3b, w=[p3b])
                                S.op("act", lambda e: e.activation(out=sm[:, 0:1], in_=p3[:, 0:1], func=AF.Abs), r=[p3b], w=[B("sm")])
                                S.op("dve", lambda e, c=c: e.tensor_tensor(out=sm[:, 1:2], in0=sm[:, 0:1], in1=ebi[:, c:c + 1], op=ALU.max), r=[B("sm"), B("ebi")], w=[B("sm")])
                                S.op("dve", lambda e: e.reciprocal(out=sm[:, 2:3], in_=sm[:, 1:2]), r=[B("sm")], w=[B("sm")])
                                S.op("act", lambda e: e.activation(out=hm[:], in_=p2[:], func=AF.Copy, scale=sm[:, 2:3]), r=[p2b, B("sm")], w=[B("hm")])
                                S.op("dve", lambda e: e.memset(sm[:, 3:4], 0.0), r=[B("sm")], w=[B("sm")])
                                S.op("act", lambda e: e.activation(out=sg[:], in_=hm[:], func=AF.Square, accum_out=sm[:, 3:4]), r=[B("hm"), B("sm")], w=[B("sg"), B("sm")])
                                S.op("dve", lambda e: e.tensor_scalar(out=sm[:, 4:5], in0=sm[:, 3:4], scalar1=1.0 / 512.0, scalar2=EPS, op0=ALU.mult, op1=ALU.add), r=[B("sm")], w=[B("sm")])
                                S.op("act", lambda e: e.activation(out=sm[:, 5:6], in_=sm[:, 4:5], func=AF.Sqrt), r=[B("sm")], w=[B("sm")])
                                S.op("dve", lambda e: e.reciprocal(out=sm[:, 6:7], in_=sm[:, 5:6]), r=[B("sm")], w=[B("sm")])
                                S.op("act", lambda e, c4=c4: e.activation(out=sg[:], in_=och[:, c4, :], func=AF.Sigmoid), r=[B("och"), B("sg")], w=[B("sg")])
                                S.op("dve", lambda e, hd=hd: e.scalar_tensor_tensor(out=y1[:], in0=hm[:], scalar=sm[:, 6:7], in1=nwb[:, hd * 512:(hd + 1) * 512], op0=ALU.mult, op1=ALU.mult),
                                     r=[B("hm"), B("sm"), B("nwb")], w=[B("y1")])
                                S.op("pool", lambda e: e.tensor_tensor(out=y2[:], in0=y1[:], in1=sg[:], op=ALU.mult), r=[B("y1"), B("sg")], w=[B("y2")])
                                for ec in range(4):
                                    S.op("pe", lambda e, ec=ec: e.transpose(PSB[:, ec * 128:(ec + 1) * 128], y2[:, ec * 128:(ec + 1) * 128], cm_b[:, IDF, :]),
                                         r=[B("y2"), B("cm_b")], w=[B("psb")])
                                S.op("act", lambda e: e.activation(out=yTb[:], in_=PSB[:], func=AF.Copy), r=[B("psb")], w=[B("yTb")])
                                oi = OWN.index(s)
                                for ec in range(4):
                                    r0 = 2048 + hd * 512 + ec * 128
                                    S.dma("sp", yT_scr[r0:r0 + 128, oi * SL + c4 * 128: oi * SL + (c4 + 1) * 128], yTb[:, ec * 128:(ec + 1) * 128],
                                          r=[B("yTb")], w=[B("yT_scr")])
                            p4, p4b = PS[6], B("ps", 6)
                            for dc in range(4):
                                S.op("pe", lambda e, dc=dc, kwi=kwi: e.matmul(p4[:, dc:dc + 1], lhsT=kw[kwi][:, dc * 128:(dc + 1) * 128], rhs=onescol_b[:], start=True, stop=True),
                                     r=[B("kw", kwi), B("onescol")], w=[p4b])
                            S.op("dve", lambda e, c=c: e.scalar_tensor_tensor(out=nst[:], in0=nst[:], scalar=dec[:, c:c + 1], in1=p4[:, 0:4], op0=ALU.mult, op1=ALU.add),
                                 r=[B("nst"), B("dec"), p4b], w=[B("nst")])
                            S.op("pool", lambda e: e.tensor_copy(out=nbf[:], in_=nst[:]), r=[B("nst")], w=[B("nbf")])
                            for dc in range(4):
                                pc_, pcb = PS[4 + dc % 2], B("ps", 4 + dc % 2)
                                S.op("pe", lambda e, dc=dc, kwi=kwi, c4=c4, pc_=pc_: e.matmul(pc_[:], lhsT=kw[kwi][:, dc * 128:(dc + 1) * 128], rhs=vch[:, c4, :], start=True, stop=True),
                                     r=[B("kw", kwi), B("vch")], w=[pcb])
                                S.op("dve", lambda e, dc=dc, c=c, pc_=pc_: e.scalar_tensor_tensor(out=Cst[:, dc, :], in0=Cst[:, dc, :], scalar=dec[:, c:c + 1], in1=pc_[:], op0=ALU.mult, op1=ALU.add),
                                     r=[B("Cst", dc), B("dec"), pcb], w=[B("Cst", dc)])
                                S.op("pool", lambda e, dc=dc: e.tensor_copy(out=Cbf[:, dc, :], in_=Cst[:, dc, :]), r=[B("Cst", dc)], w=[B("Cbf")])
            S.barrier()
        if on("p4"):
            with ExitStack() as ps_:
                sbp = lambda name, shape, dtype=F32: ps_.enter_context(nc.sbuf_tensor(name, list(shape), dtype))
                PS = [ps_.enter_context(nc.psum_tensor("p4ps%d" % i, [128, 512], F32)) for i in range(7)]
                PSB = ps_.enter_context(nc.psum_tensor("p4psb", [128, 512], BF16))
                yT = sbp("yT", [128, KC, SL], BF16)
                Wo = [sbp("Wo%d" % i, [128, KC, 256], BF16) for i in range(3)]
                hT = sbp("hT", [128, KC, SL])
                xc = [sbp("xc%d" % i, [128, SL]) for i in range(3)]
                sq = [sbp("sq4_%d" % i, [128, SL], BF16) for i in range(3)]
                rstd = sbp("rstd4", [128, SL])
                tmp = [sbp("tmp4_%d" % i, [128, SL]) for i in range(3)]
                u2f = [sbp("u2f%d" % i, [128, SL]) for i in range(3)]
                u2b = [sbp("u2b%d" % i, [128, SL], BF16) for i in range(3)]
                wrt = sbp("wrt", [128, KC, 36])
                brt = sbp("brt", [128, 36])
                L = sbp("L", [128, 36])
                r8 = sbp("r8", [128, 16])
                ohg = sbp("ohg", [128, 4])
                eg = sbp("eg", [128, 4])
                es_ = sbp("es_", [128, 8])
                e2 = sbp("e2", [128, 8])
                oh1 = sbp("oh1", [128, 8])
                oh2 = sbp("oh2", [128, 8])
                gw8 = sbp("gw8", [128, 8])
                gw32 = sbp("gw32", [128, 32])
                S.dma("sp", wrt[:], wr.rearrange("(c p) n -> p c n", p=128), w=[B("wrt")])
                S.dma("sp", brt[:], br[:], w=[B("brt")])
                woi = [0]
                for oi, so in enumerate(OWN):
                    osl = slice(oi * SL, (oi + 1) * SL)
                    S.dma("sp", yT[:], yT_scr[:, osl].rearrange("(c p) t -> p c t", p=128), r=[B("yT_scr")], w=[B("yT")])
                    for g in range(16):
                        k = woi[0] % 3
                        woi[0] += 1
                        S.dma("pool", Wo[k][:], w_out[:, g * 256:(g + 1) * 256].rearrange("(c p) n -> p c n", p=128), w=[B("Wo", k)])
                        for cc in range(2):
                            dc = g * 2 + cc
                            pt, pb = PS[dc % 4], B("ps", dc % 4)
                            for kc in range(KC):
                                S.op("pe", lambda e, kc=kc, cc=cc, pt=pt, k=k: e.matmul(pt[:], lhsT=Wo[k][:, kc, cc * 128:(cc + 1) * 128], rhs=yT[:, kc, :],
                                                                                  start=(kc == 0), stop=(kc == KC - 1)),
                                     r=[B("Wo", k), B("yT")], acc=pb, w=[pb], inc=(kc == KC - 1))
                            xi = dc % 3
                            S.dma("act", xc[xi][:], xs[dc * 128:(dc + 1) * 128, so * SL:(so + 1) * SL], w=[B("xc", xi)])
                            S.op("dve", lambda e, dc=dc, pt=pt, xi=xi: e.scalar_tensor_tensor(out=hT[:, dc, :], in0=pt[:], scalar=mod[:, 2, dc:dc + 1], in1=xc[xi][:], op0=ALU.mult, op1=ALU.add),
                                 r=[pb, B("mod"), B("xc", xi)], w=[B("hT", dc)])
                            S.dma("sp", hT_scr[dc * 128:(dc + 1) * 128, osl], hT[:, dc, :], r=[B("hT", dc)], w=[B("hT_scr")])
                    pt, pb = PS[4], B("ps", 4)
                    for kc in range(KC):
                        qi = kc % 3
                        S.op("act", lambda e, qi=qi, kc=kc: e.activation(out=sq[qi][:], in_=hT[:, kc, :], func=AF.Square), r=[B("hT", kc)], w=[B("sq4", qi)])
                        S.op("pe", lambda e, qi=qi, kc=kc: e.matmul(pt[:], lhsT=cm_b[:, ONE, :], rhs=sq[qi][:], start=(kc == 0), stop=(kc == KC - 1)),
                             r=[B("sq4", qi), B("cm_b")], acc=pb, w=[pb], inc=True)
                    S.op("dve", lambda e: e.tensor_scalar(out=rstd[:], in0=pt[:], scalar1=1.0 / D, scalar2=EPS, op0=ALU.mult, op1=ALU.add), r=[pb], w=[B("rstd4")])
                    S.op("act", lambda e: e.activation(out=rstd[:], in_=rstd[:], func=AF.Sqrt), r=[B("rstd4")], w=[B("rstd4")])
                    S.op("dve", lambda e: e.reciprocal(out=rstd[:], in_=rstd[:]), r=[B("rstd4")], w=[B("rstd4")])
                    lp, lpb = PS[5], B("ps", 5)
                    for kc in range(KC):
                        ti = kc % 3
                        S.op("dve", lambda e, ti=ti, kc=kc: e.tensor_tensor(out=tmp[ti][:], in0=hT[:, kc, :], in1=rstd[:], op=ALU.mult),
                             r=[B("hT", kc), B("rstd4")], w=[B("tmp4", ti)])
                        S.op("act", lambda e, ti=ti, kc=kc: e.activation(out=u2f[ti][:], in_=tmp[ti][:], func=AF.Identity, scale=AB[:, 2, kc:kc + 1], bias=AB[:, 3, kc:kc + 1]),
                             r=[B("tmp4", ti), B("AB")], w=[B("u2f", ti)])
                        S.op("pool", lambda e, ti=ti: e.tensor_copy(out=u2b[ti][:], in_=u2f[ti][:]), r=[B("u2f", ti)], w=[B("u2b", ti)])
                        S.dma("sp", u2T_scr[kc * 128:(kc + 1) * 128, osl], u2b[ti][:], r=[B("u2b", ti)], w=[B("u2T_scr")])
                        for tb in range(4):
                            S.op("pe", lambda e, ti=ti, kc=kc, tb=tb: e.matmul(PS[tb][:, 0:36], lhsT=u2f[ti][:, tb * 128:(tb + 1) * 128], rhs=wrt[:, kc, :],
                                                                           start=(kc == 0), stop=(kc == KC - 1)),
                                 r=[B("u2f", ti), B("wrt")], acc=B("ps", tb), w=[B("ps", tb)], inc=(tb == 3))
                    for tb in range(4):
                        dv = lambda fn, r, w: S.op("dve", fn, r=r, w=w)
                        RB = [B("rt")]
                        dv(lambda e, tb=tb: e.tensor_tensor(out=L[:], in0=PS[tb][:, 0:36], in1=brt[:], op=ALU.add), [B("ps", tb), B("brt")] + RB, RB)
                        dv(lambda e: e.tensor_reduce(out=r8[:, 0:1], in_=L[:, 0:4], axis=AX.X, op=ALU.max), RB, RB)
                        dv(lambda e: e.tensor_scalar(out=r8[:, 1:2], in0=r8[:, 0:1], scalar1=-1.0, scalar2=None, op0=ALU.mult), RB, RB)
                        S.op("act", lambda e: e.activation(out=eg[:], in_=L[:, 0:4], func=AF.Exp, bias=r8[:, 1:2]), r=RB, w=RB)
                        dv(lambda e: e.tensor_reduce(out=r8[:, 2:3], in_=eg[:], axis=AX.X, op=ALU.add), RB, RB)
                        dv(lambda e: e.reciprocal(out=r8[:, 3:4], in_=r8[:, 2:3]), RB, RB)
                        dv(lambda e: e.tensor_scalar(out=ohg[:], in0=L[:, 0:4], scalar1=r8[:, 0:1], scalar2=None, op0=ALU.is_equal), RB, RB)
                        dv(lambda e: e.tensor_scalar(out=es_[:], in0=L[:, 4:12], scalar1=ohg[:, 0:1], scalar2=None, op0=ALU.mult), RB, RB)
                        for g in range(1, 4):
                            dv(lambda e, g=g: e.scalar_tensor_tensor(out=es_[:], in0=L[:, 4 + g * 8: 12 + g * 8], scalar=ohg[:, g:g + 1], in1=es_[:], op0=ALU.mult, op1=ALU.add), RB, RB)
                        dv(lambda e: e.tensor_reduce(out=r8[:, 4:5], in_=es_[:], axis=AX.X, op=ALU.max), RB, RB)
                        dv(lambda e: e.tensor_scalar(out=oh1[:], in0=es_[:], scalar1=r8[:, 4:5], scalar2=None, op0=ALU.is_equal), RB, RB)
                        dv(lambda e: e.scalar_tensor_tensor(out=e2[:], in0=oh1[:], scalar=-1e30, in1=es_[:], op0=ALU.mult, op1=ALU.add), RB, RB)
                        dv(lambda e: e.tensor_reduce(out=r8[:, 5:6], in_=e2[:], axis=AX.X, op=ALU.max), RB, RB)
                        dv(lambda e: e.tensor_scalar(out=oh2[:], in0=e2[:], scalar1=r8[:, 5:6], scalar2=None, op0=ALU.is_equal), RB, RB)
                        dv(lambda e: e.tensor_tensor(out=r8[:, 6:7], in0=r8[:, 5:6], in1=r8[:, 4:5], op=ALU.subtract), RB, RB)
                        S.op("act", lambda e: e.activation(out=r8[:, 7:8], in_=r8[:, 6:7], func=AF.Exp), r=RB, w=RB)
                        dv(lambda e: e.tensor_scalar(out=r8[:, 8:9], in0=r8[:, 7:8], scalar1=1.0, scalar2=None, op0=ALU.add), RB, RB)
                        dv(lambda e: e.reciprocal(out=r8[:, 9:10], in_=r8[:, 8:9]), RB, RB)
                        dv(lambda e: e.tensor_tensor(out=r8[:, 10:11], in0=r8[:, 9:10], in1=r8[:, 3:4], op=ALU.mult), RB, RB)
                        dv(lambda e: e.tensor_tensor(out=r8[:, 11:12], in0=r8[:, 10:11], in1=r8[:, 7:8], op=ALU.mult), RB, RB)
                        dv(lambda e: e.tensor_scalar(out=gw8[:], in0=oh1[:], scalar1=r8[:, 10:11], scalar2=None, op0=ALU.mult), RB, RB)
                        dv(lambda e: e.scalar_tensor_tensor(out=gw8[:], in0=oh2[:], scalar=r8[:, 11:12], in1=gw8[:], op0=ALU.mult, op1=ALU.add), RB, RB)
                        for g in range(4):
                            dv(lambda e, g=g: e.tensor_scalar(out=gw32[:, g * 8:(g + 1) * 8], in0=gw8[:], scalar1=ohg[:, g:g + 1], scalar2=None, op0=ALU.mult), RB, RB)
                        tp, tpb = PS[6], B("ps", 6)
                        S.op("pe", lambda e: e.transpose(tp[0:32, 0:128], gw32[:], cm_f[:, IDF, :]), r=RB + [B("cm_f")], w=[tpb])
                        dv(lambda e, tb=tb, oi=oi: e.tensor_copy(out=gwT[:, oi * SL + tb * 128: oi * SL + (tb + 1) * 128], in_=tp[0:32, 0:128]), [tpb], [B("gwT")])
            S.barrier()
        if "gwT" in dbg:
            S.dma("sp", dbg["gwT"], gwT[:], r=[B("gwT")])

        if on("p5"):
            for tile_ in range(2):
                tsl = slice(tile_ * 1024, (tile_ + 1) * 1024)
                with ExitStack() as ps_:
                    sbp = lambda name, shape, dtype=F32: ps_.enter_context(nc.sbuf_tensor(name + "_t%d" % tile_, list(shape), dtype))
                    PS = [ps_.enter_context(nc.psum_tensor("p5aps%d_%d" % (tile_, i), [128, 512], F32)) for i in range(7)]
                    u2 = sbp("u2", [128, KC, 1024], BF16)
                    Wg = [sbp("Wg%d" % i, [128, KC, 256], BF16) for i in range(2)]
                    Wu = [sbp("Wu%d" % i, [128, KC, 256], BF16) for i in range(2)]
                    Wst = [sbp("Wst%d" % i, [128, 16, 256]) for i in range(2)]
                    gwb = [sbp("gwb%d" % i, [128, 1024]) for i in range(2)]
                    gm = [sbp("gm%d" % i, [32, 1024]) for i in range(2)]
                    sgl = [sbp("sgl%d" % i, [128, SL]) for i in range(2)]
                    tu = [sbp("tu%d" % i, [128, SL]) for i in range(2)]
                    actS = [sbp("actS%d" % i, [128, 2, 1024], BF16) for i in range(2)]
                    S.dma("sp", u2[:], u2T_scr[:, tsl].rearrange("(c p) t -> p c t", p=128), r=[B("u2T_scr")], w=[B("u2")])
                    rot = [0, 0, 0, 0]
                    for ex in range(NEXP):
                        gi = ex % 2
                        S.op("dve", lambda e, ex=ex, gi=gi: e.tensor_scalar(out=gm[gi][:], in0=gwT[:, tsl], scalar1=cm_f[0:32, IDF, ex:ex + 1], scalar2=None, op0=ALU.mult),
                             r=[B("gwT"), B("cm_f")], w=[B("gm", gi)])
                        for half in range(2):
                            bp, bpb = PS[6], B("ps", 6)
                            S.op("pe", lambda e, gi=gi, half=half: e.matmul(bp[:], lhsT=cm_f[0:32, ONE, :], rhs=gm[gi][:, half * SL:(half + 1) * SL], start=True, stop=True),
                                 r=[B("gm", gi), B("cm_f")], w=[bpb])
                            S.op("act", lambda e, gi=gi, half=half: e.activation(out=gwb[gi][:, half * SL:(half + 1) * SL], in_=bp[:], func=AF.Copy), r=[bpb], w=[B("gwb", gi)])
                        for fp in range(4):
                            k = rot[0] % 2
                            rot[0] += 1
                            for (wsrc, wdst, wname) in ((wg, Wg, "Wg"), (wu, Wu, "Wu")):
                                for kh in range(2):
                                    si_ = rot[3] % 2
                                    rot[3] += 1
                                    S.dma("sp" if si_ else "act", Wst[si_][:], wsrc[ex, kh * 2048:(kh + 1) * 2048, fp * 256:(fp + 1) * 256].rearrange("(c p) n -> p c n", p=128), w=[B("Wst", si_)])
                                    if si_:
                                        S.op("pool", lambda e, k=k, kh=kh, si_=si_, wdst=wdst: e.tensor_copy(out=wdst[k][:, kh * 16:(kh + 1) * 16, :], in_=Wst[si_][:]), r=[B("Wst", si_)], w=[B(wname, k)])
                                    else:
                                        S.op("act", lambda e, k=k, kh=kh, si_=si_, wdst=wdst: e.activation(out=wdst[k][:, kh * 16:(kh + 1) * 16, :], in_=Wst[si_][:], func=AF.Copy), r=[B("Wst", si_)], w=[B(wname, k)])
                            ai = rot[1] % 2
                            rot[1] += 1
                            for f2 in range(2):
                                for half in range(2):
                                    r3 = rot[2] % 3
                                    t2 = rot[2] % 2
                                    rot[2] += 1
                                    gp, gpb = PS[2 * r3], B("ps", 2 * r3)
                                    up, upb = PS[2 * r3 + 1], B("ps", 2 * r3 + 1)
                                    hs = slice(half * SL, (half + 1) * SL)
                                    for kc in range(KC):
                                        S.op("pe", lambda e, kc=kc, k=k, gp=gp, f2=f2, hs=hs: e.matmul(gp[:], lhsT=Wg[k][:, kc, f2 * 128:(f2 + 1) * 128], rhs=u2[:, kc, hs], start=(kc == 0), stop=(kc == KC - 1)),
                                             r=[B("Wg", k), B("u2")], acc=gpb, w=[gpb], inc=(kc == KC - 1))
                                    for kc in range(KC):
                                        S.op("pe", lambda e, kc=kc, k=k, up=up, f2=f2, hs=hs: e.matmul(up[:], lhsT=Wu[k][:, kc, f2 * 128:(f2 + 1) * 128], rhs=u2[:, kc, hs], start=(kc == 0), stop=(kc == KC - 1)),
                                             r=[B("Wu", k), B("u2")], acc=upb, w=[upb], inc=(kc == KC - 1))
                                    S.op("act", lambda e, t2=t2, gp=gp: e.activation(out=sgl[t2][:], in_=gp[:], func=AF.Silu), r=[gpb], w=[B("sgl", t2)])
                                    S.op("dve", lambda e, t2=t2, up=up, gi=gi, hs=hs: e.tensor_tensor(out=tu[t2][:], in0=up[:], in1=gwb[gi][:, hs], op=ALU.mult), r=[upb, B("gwb", gi)], w=[B("tu", t2)])
                                    S.op("pool", lambda e, t2=t2, ai=ai, f2=f2, hs=hs: e.tensor_tensor(out=actS[ai][:, f2, hs], in0=tu[t2][:], in1=sgl[t2][:], op=ALU.mult),
                                         r=[B("tu", t2), B("sgl", t2)], w=[B("actS", ai)])
                            S.dma("sp", act_scr[ex, fp * 256:(fp + 1) * 256, :].rearrange("(c p) t -> p c t", p=128), actS[ai][:], r=[B("actS", ai)], w=[B("act_scr")])
                S.barrier()
                with ExitStack() as ps_:
                    sbp = lambda name, shape, dtype=F32: ps_.enter_context(nc.sbuf_tensor(name + "_t%d" % tile_, list(shape), dtype))
                    PS = [ps_.enter_context(nc.psum_tensor("p5bps%d_%d" % (tile_, i), [128, 512], F32)) for i in range(7)]
                    acc = sbp("acc", [128, KC, 1024])
                    actT = sbp("actT", [128, 8, 1024], BF16)
                    Wd = [sbp("Wd%d" % i, [128, 8, 512], BF16) for i in range(2)]
                    hx = [sbp("hx%d" % i, [128, SL]) for i in range(2)]
                    sq = [sbp("sq5_%d" % i, [128, SL], BF16) for i in range(2)]
                    rstd = sbp("rstd5", [128, SL])
                    tmp = [sbp("tmp5_%d" % i, [128, SL]) for i in range(2)]
                    ot = [sbp("ot%d" % i, [128, SL]) for i in range(2)]
                    S.op("pool", lambda e: e.memset(acc[:], 0.0), w=[B("acc", d_, h_) for d_ in range(KC) for h_ in range(2)])
                    cnt = [0, 0]
                    for ex in range(NEXP):
                        S.dma("sp", actT[:], act_scr[ex].rearrange("(c p) t -> p c t", p=128), r=[B("act_scr")], w=[B("actT")])
                        for dg in range(8):
                            k = cnt[0] % 2
                            cnt[0] += 1
                            S.dma("pool", Wd[k][:], wd[ex, :, dg * 512:(dg + 1) * 512].rearrange("(c p) n -> p c n", p=128), w=[B("Wd", k)])
                            for d4 in range(4):
                                dc = dg * 4 + d4
                                for half in range(2):
                                    pi = cnt[1] % 6
                                    cnt[1] += 1
                                    dp, dpb = PS[pi], B("ps", pi)
                                    hs = slice(half * SL, (half + 1) * SL)
                                    for fc in range(8):
                                        S.op("pe", lambda e, fc=fc, k=k, d4=d4, dp=dp, hs=hs: e.matmul(dp[:], lhsT=Wd[k][:, fc, d4 * 128:(d4 + 1) * 128], rhs=actT[:, fc, hs], start=(fc == 0), stop=(fc == 7)),
                                             r=[B("Wd", k), B("actT")], acc=dpb, w=[dpb], inc=(fc == 7))
                                    S.op("dve", lambda e, dc=dc, dp=dp, hs=hs: e.tensor_tensor(out=acc[:, dc, hs], in0=acc[:, dc, hs], in1=dp[:], op=ALU.add),
                                         r=[dpb, B("acc", dc, half)], w=[B("acc", dc, half)])
                    for half in range(2):
                        oi = 2 * tile_ + half
                        osl = slice(oi * SL, (oi + 1) * SL)
                        hs = slice(half * SL, (half + 1) * SL)
                        pt, pb = PS[6], B("ps", 6)
                        for kc in range(KC):
                            hi = kc % 2
                            S.dma("sp", hx[hi][:], hT_scr[kc * 128:(kc + 1) * 128, osl], r=[B("hT_scr")], w=[B("hx", hi)])
                            S.op("dve", lambda e, kc=kc, hi=hi, hs=hs: e.scalar_tensor_tensor(out=acc[:, kc, hs], in0=acc[:, kc, hs], scalar=mod[:, 5, kc:kc + 1], in1=hx[hi][:], op0=ALU.mult, op1=ALU.add),
                                 r=[B("acc", kc, half), B("mod"), B("hx", hi)], w=[B("acc", kc, half)])
                            qi = kc % 2
                            S.op("act", lambda e, qi=qi, kc=kc, hs=hs: e.activation(out=sq[qi][:], in_=acc[:, kc, hs], func=AF.Square), r=[B("acc", kc, half)], w=[B("sq5", qi)])
                            S.op("pe", lambda e, qi=qi, kc=kc: e.matmul(pt[:], lhsT=cm_b[:, ONE, :], rhs=sq[qi][:], start=(kc == 0), stop=(kc == KC - 1)),
                                 r=[B("sq5", qi), B("cm_b")], acc=pb, w=[pb], inc=True)
                        S.op("dve", lambda e: e.tensor_scalar(out=rstd[:], in0=pt[:], scalar1=1.0 / D, scalar2=EPS, op0=ALU.mult, op1=ALU.add), r=[pb], w=[B("rstd5")])
                        S.op("act", lambda e: e.activation(out=rstd[:], in_=rstd[:], func=AF.Sqrt), r=[B("rstd5")], w=[B("rstd5")])
                        S.op("dve", lambda e: e.reciprocal(out=rstd[:], in_=rstd[:]), r=[B("rstd5")], w=[B("rstd5")])
                        for kc in range(KC):
                            ti = kc % 2
                            S.op("dve", lambda e, ti=ti, kc=kc, hs=hs: e.tensor_tensor(out=tmp[ti][:], in0=acc[:, kc, hs], in1=rstd[:], op=ALU.mult),
                                 r=[B("acc", kc, half), B("rstd5")], w=[B("tmp5", ti)])
                            S.op("act", lambda e, ti=ti, kc=kc: e.activation(out=ot[ti][:], in_=tmp[ti][:], func=AF.Identity, scale=AB[:, 4, kc:kc + 1], bias=AB[:, 5, kc:kc + 1]),
                                 r=[B("tmp5", ti), B("AB")], w=[B("ot", ti)])
                            S.dma("sp", outT[kc * 128:(kc + 1) * 128, osl], ot[ti][:], r=[B("ot", ti)], w=[B("outT")])
                S.barrier()
        for nm in debug:
            if nm in SCR:
                S.dma("sp", dbg[nm], SCR[nm][0], r=[B(nm)])
        S.barrier()
    return nc, S, EXT


def _pc(v):
    return np.ascontiguousarray(np.asarray(v, np.float32).reshape(-1, 128).T)


def kernel(x, c, norm1_w, w_in, conv_w, ml_gate_bias, ml_norm_w, w_out, norm2_w,
           w_router_group, b_router_group, w_router_expert, b_router_expert,
           w_exp_gate, w_exp_up, w_exp_down, w_ada, b_ada, final_norm_w,
           w_ada_final, b_ada_final, _phases=("all",), _debug=(), _cores=None):
    f = lambda a: np.ascontiguousarray(np.asarray(a, dtype=np.float32))
    x = f(x)
    nc, S, EXT = build(phases=_phases, debug=_debug)
    j_ = np.arange(128)
    cmat = np.zeros((128, 5, 128), np.float32)
    cmat[:, 0, :] = np.eye(128)
    cmat[:, 1, :] = (j_[:, None] >= j_[None, :])
    cmat[:, 2, :] = (j_[:, None] < j_[None, :])
    cmat[:, 3, :] = (j_[:, None] <= j_[None, :])
    cmat[:, 4, :] = 1.0
    t_ = np.arange(SL)
    cmask = np.zeros((128, 4, SL), np.float32)
    for m in range(4):
        cmask[:, m, :] = ((j_[:, None] + 128 * m) < t_[None, :])
    csel = np.zeros((32, NEXP, 128), np.float32)
    for e in range(NEXP):
        csel[e, e, :] = 1.0
    vecs = np.zeros((128, 8, KC), np.float32)
    vecs[:, 0, :] = _pc(norm1_w[0])
    vecs[:, 1, :] = _pc(norm2_w[0])
    vecs[:, 2, :] = _pc(final_norm_w)
    bada = np.zeros((128, 8, KC), np.float32)
    ba = np.asarray(b_ada[0], np.float32)
    for v in range(6):
        bada[:, v, :] = _pc(ba[v * D:(v + 1) * D])
    baf = np.asarray(b_ada_final, np.float32)
    for v in range(2):
        bada[:, 6 + v, :] = _pc(baf[v * D:(v + 1) * D])
    cw = np.asarray(conv_w[0], np.float32)
    convw = np.stack([_pc(cw[i]) for i in range(4)], axis=1)
    gbias = np.ascontiguousarray(np.broadcast_to(np.asarray(ml_gate_bias[0], np.float32)[None, :], (128, 8)))
    mlnw = np.ascontiguousarray(np.broadcast_to(np.asarray(ml_norm_w[0], np.float32)[None, :], (128, 2048)))
    wr = np.ascontiguousarray(np.concatenate([np.asarray(w_router_group[0], np.float32), np.asarray(w_router_expert[0], np.float32)], axis=1))
    brv = np.concatenate([np.asarray(b_router_group[0], np.float32), np.asarray(b_router_expert[0], np.float32)])
    br = np.ascontiguousarray(np.broadcast_to(brv[None, :], (128, 36)))
    shared = dict(vecs=vecs, bada=bada, w_in=f(w_in[0]), w_out=f(w_out[0]), w_ada=f(w_ada[0]), w_adaf=f(w_ada_final),
                  convw=np.ascontiguousarray(convw), gbias=gbias, mlnw=mlnw, wr=wr, br=br,
                  wg=f(w_exp_gate[0]), wu=f(w_exp_up[0]), wd=f(w_exp_down[0]), cmat=cmat, cmask=cmask)
    in_maps = []
    for core in range(8):
        b, j = core // 4, core % 4
        xs = np.zeros((D, NSLOT * SL), np.float32)
        sv = np.zeros((128, NSLOT), np.float32)
        for s in range(NSLOT):
            g = s + j - 3
            if g >= 0:
                xs[:, s * SL:(s + 1) * SL] = x[b, g * SL:(g + 1) * SL, :].T
                sv[:, s] = 1.0
        m = dict(shared)
        m["xs"] = xs
        m["slotvalid"] = sv
        m["cpc"] = _pc(c[b])
        in_maps.append({k: v for k, v in m.items() if k in EXT})
    if _cores is not None:
        in_maps = [in_maps[i] for i in _cores]
        return run_bass_kernel_spmd(nc, in_maps, core_ids=list(range(len(_cores))))
    res = run_bass_kernel_spmd(nc, in_maps, core_ids=list(range(8)))
    out = np.empty((2, 8192, D), np.float32)
    for core in range(8):
        b, j = core // 4, core % 4
        oT = np.asarray(res.results[core]["outT"])
        for oi in range(4):
            g = j + 4 * oi
            out[b, g * SL:(g + 1) * SL, :] = oT[:, oi * SL:(oi + 1) * SL].T
    return out
```

```python
import os as _os
import numpy as np
from contextlib import ExitStack
import concourse.bass as bass
import concourse.mybir as mybir
from concourse.bass_utils import run_bass_kernel_spmd

F32 = mybir.dt.float32
BF16 = mybir.dt.bfloat16
AF = mybir.ActivationFunctionType
ALU = mybir.AluOpType
AX = mybir.AxisListType

D = 4096
KC = 32
NSLOT = 16
SL = 512
OWN = [3, 7, 11, 15]
NEXP = 32
DFF = 1024
EPS = 1e-6
PROJ = 14344


class Buf:
    __slots__ = ("name", "w", "r")

    def __init__(self, name):
        self.name = name
        self.w = None
        self.r = []


class Eng:
    def __init__(self, name, eng, sem):
        self.name = name
        self.eng = eng
        self.sem = sem
        self.count = 0
        self.ins = []
        self.waited = {}


class Sched:
    def __init__(self, nc, es, ndma=12):
        self.nc = nc
        self.E = {}
        for nm, e in (("pe", nc.tensor), ("act", nc.scalar), ("dve", nc.vector), ("pool", nc.gpsimd), ("sp", nc.sync)):
            self.E[nm] = Eng(nm, e, es.enter_context(nc.semaphore("sem_" + nm)))
        self.dsem = {}
        for q in ("sp", "pool", "act"):
            self.dsem[q] = [[es.enter_context(nc.semaphore("dq_%s_%d" % (q, i))), 0] for i in range(ndma)]
        self.dnext = {"sp": 0, "pool": 0, "act": 0}
        self.bufs = {}
        self.all_dma_tokens = []

    def B(self, *key):
        b = self.bufs.get(key)
        if b is None:
            b = Buf(key)
            self.bufs[key] = b
        return b

    def _resolve(self, tok):
        if tok[0] == "sem":
            return tok[1], tok[2]
        e, idx = tok[1], tok[2]
        for k in range(idx, len(e.ins)):
            if e.ins[k][1] is not None:
                return e.sem, e.ins[k][1]
        last = e.ins[-1]
        e.count += 1
        last[0].then_inc(e.sem, 1)
        last[1] = e.count
        return e.sem, e.count

    def _wait(self, eng, toks):
        need = {}
        for t in toks:
            if t is None:
                continue
            sem, val = self._resolve(t)
            k = id(sem)
            if eng.waited.get(k, 0) >= val:
                continue
            if k not in need or need[k][1] < val:
                need[k] = (sem, val)
        for k, (sem, val) in need.items():
            eng.eng.wait_ge(sem, val)
            eng.waited[k] = val

    def _deps(self, r, w, skip_pe_acc=None):
        toks = []
        for b in r:
            toks.append(b.w)
        for b in w:
            if b is skip_pe_acc:
                continue
            toks.append(b.w)
            toks.extend(b.r)
        return toks

    def op(self, en, fn, r=(), w=(), inc=True, acc=None):
        e = self.E[en]
        toks = self._deps(r, w, skip_pe_acc=acc)
        if acc is not None:
            toks.extend(t for t in acc.r)
            if acc.w is not None and not (acc.w[0] == "eng" and acc.w[1] is e):
                toks.append(acc.w)
        self._wait(e, toks)
        ins = fn(e.eng)
        ent = [ins, None]
        if inc:
            e.count += 1
            ins.then_inc(e.sem, 1)
            ent[1] = e.count
        e.ins.append(ent)
        tok = ("eng", e, len(e.ins) - 1)
        for b in r:
            b.r.append(tok)
        for b in w:
            b.w = tok
            b.r = []
        return tok

    def dma(self, q, out, in_, r=(), w=()):
        e = self.E[q]
        toks = self._deps(r, w)
        slot = self.dsem[q][self.dnext[q] % len(self.dsem[q])]
        self.dnext[q] += 1
        sem, cnt = slot
        if cnt > 0:
            toks.append(("sem", sem, cnt * 16))
        self._wait(e, toks)
        e.eng.dma_start(out=out, in_=in_).then_inc(sem, 16)
        slot[1] = cnt + 1
        tok = ("sem", sem, (cnt + 1) * 16)
        for b in r:
            b.r.append(tok)
        for b in w:
            b.w = tok
            b.r = []
        self.all_dma_tokens.append(tok)
        return tok

    def barrier(self):
        toks = []
        for e in self.E.values():
            if e.ins:
                toks.append(("eng", e, len(e.ins) - 1))
        for q in self.dsem:
            for sem, cnt in self.dsem[q]:
                if cnt > 0:
                    toks.append(("sem", sem, cnt * 16))
        for e in self.E.values():
            self._wait(e, toks)
        for b in self.bufs.values():
            b.w = None
            b.r = []


def build(phases=("all",), debug=()):
    nc = bass.Bass("TRN2", target_bir_lowering=False)
    dt = nc.dram_tensor

    SPECS = dict(xs=[D, NSLOT * SL], cpc=[128, KC], vecs=[128, 8, KC], bada=[128, 8, KC], slotvalid=[128, NSLOT],
                 w_in=[D, PROJ], w_out=[D, D], w_ada=[D, 6 * D], w_adaf=[D, 2 * D], convw=[128, 4, KC], gbias=[128, 8],
                 mlnw=[128, 2048], wr=[D, 36], br=[128, 36], wg=[NEXP, D, DFF], wu=[NEXP, D, DFF], wd=[NEXP, DFF, D],
                 cmat=[128, 5, 128], cmask=[128, 4, SL])
    EXT = {}

    class _Lazy:
        def __init__(self, name):
            self.name = name

        def _ap(self):
            if self.name not in EXT:
                EXT[self.name] = dt(self.name, list(SPECS[self.name]), F32, kind="ExternalInput").ap()
            return EXT[self.name]

        def __getitem__(self, k):
            return self._ap()[k]

        def rearrange(self, *a, **k):
            return self._ap().rearrange(*a, **k)

    xs, cpc, vecs, bada, slotvalid, w_in, w_out, w_ada, w_adaf, convw, gbias, mlnw, wr, br, wg, wu, wd, cmat, cmask = [
        _Lazy(n) for n in ("xs", "cpc", "vecs", "bada", "slotvalid", "w_in", "w_out", "w_ada", "w_adaf", "convw", "gbias",
                           "mlnw", "wr", "br", "wg", "wu", "wd", "cmat", "cmask")]
    outT = dt("outT", [D, 4 * SL], F32, kind="ExternalOutput").ap()

    NT = NSLOT * SL
    qT_scr = dt("qT_scr", [2048, 4 * SL], BF16).ap()
    kT_scr = dt("kT_scr", [2048, NT], BF16).ap()
    v_scr = dt("v_scr", [16, 128, 64, 128], BF16).ap()
    mlk_scr = dt("mlk_scr", [2048, NT + 8], BF16).ap()
    mlq_scr = dt("mlq_scr", [2048, 4 * 520], BF16).ap()
    mlv_scr = dt("mlv_scr", [4, 128, 64, 512], BF16).ap()
    mlo_scr = dt("mlo_scr", [4, 128, 16, 512], BF16).ap()
    if_scr = dt("if_scr", [128, 64, 8], F32).ap()
    yT_scr = dt("yT_scr", [D, 4 * SL], BF16).ap()
    hT_scr = dt("hT_scr", [D, 4 * SL], F32).ap()
    u2T_scr = dt("u2T_scr", [D, 4 * SL], BF16).ap()
    act_scr = dt("act_scr", [NEXP, DFF, 1024], BF16).ap()
    dbg = {}
    SCR = dict(qT_scr=(qT_scr, BF16), kT_scr=(kT_scr, BF16), v_scr=(v_scr, BF16), mlk_scr=(mlk_scr, BF16), mlq_scr=(mlq_scr, BF16),
               mlv_scr=(mlv_scr, BF16), mlo_scr=(mlo_scr, BF16), if_scr=(if_scr, F32), yT_scr=(yT_scr, BF16), hT_scr=(hT_scr, F32),
               u2T_scr=(u2T_scr, BF16))
    SBD = dict(mod=[128, 8, KC], AB=[128, 6, KC], gwT=[32, 4 * SL])
    for nm in debug:
        if nm in SCR:
            dbg[nm] = dt("dbg_" + nm, list(SCR[nm][0].shape), SCR[nm][1], kind="ExternalOutput").ap()
        else:
            dbg[nm] = dt("dbg_" + nm, SBD[nm], F32, kind="ExternalOutput").ap()

    def on(p):
        return "all" in phases or p in phases

    with ExitStack() as es:
        S = Sched(nc, es)
        B = S.B
        sb = lambda name, shape, dtype=F32: es.enter_context(nc.sbuf_tensor(name, list(shape), dtype))
        cm_f = sb("cm_f", [128, 5, 128])
        cm_b = sb("cm_b", [128, 5, 128], BF16)
        onescol_b = sb("onescol_b", [128, 1], BF16)
        mod = sb("mod", [128, 8, KC])
        vec = sb("vec", [128, 8, KC])
        AB = sb("AB", [128, 6, KC])
        sval = sb("sval", [128, NSLOT])
        gwT = sb("gwT", [32, 4 * SL])
        IDF, UTm, LTm, LEm, ONE = 0, 1, 2, 3, 4

        S.dma("sp", cm_f[:], cmat[:], w=[B("cm_f")])
        S.dma("pool", cm_b[:], cmat[:], w=[B("cm_b")])
        S.dma("sp", vec[:], vecs[:], w=[B("vec")])
        S.dma("sp", sval[:], slotvalid[:], w=[B("sval")])
        S.op("dve", lambda e: e.memset(onescol_b[:], 1.0), w=[B("onescol")])

        if on("p0"):
            with ExitStack() as ps_:
                sbp = lambda name, shape, dtype=F32: ps_.enter_context(nc.sbuf_tensor(name, list(shape), dtype))
                PS = [ps_.enter_context(nc.psum_tensor("p0ps%d" % i, [128, 512], F32)) for i in range(7)]
                PSB = ps_.enter_context(nc.psum_tensor("p0psb", [128, 512], BF16))
                cf = sbp("cf", [128, KC])
                cb = sbp("cb", [128, KC], BF16)
                bad = sbp("bad", [128, 8, KC])
                wa = [sbp("wa%d" % i, [128, KC, 512], BF16) for i in range(2)]
                S.dma("sp", cf[:], cpc[:], w=[B("cf")])
                S.dma("sp", bad[:], bada[:], w=[B("bad")])
                S.op("act", lambda e: e.activation(out=cb[:], in_=cf[:], func=AF.Silu), r=[B("cf")], w=[B("cb")])
                gi = 0
                for v in range(8):
                    src = w_ada if v < 6 else w_adaf
                    c0 = (v if v < 6 else v - 6) * D
                    pb = B("ps", v % 2)
                    pt = PS[v % 2]
                    for g in range(8):
                        wt = wa[gi % 2]
                        wb_ = B("wa", gi % 2)
                        gi += 1
                        S.dma("pool", wt[:], src[:, c0 + g * 512: c0 + (g + 1) * 512].rearrange("(c p) n -> p c n", p=128), w=[wb_])
                        for cc in range(4):
                            col = g * 4 + cc
                            for kc in range(KC):
                                S.op("pe", lambda e, wt=wt, cc=cc, kc=kc, col=col, pt=pt: e.matmul(
                                    pt[:, col:col + 1], lhsT=wt[:, kc, cc * 128:(cc + 1) * 128], rhs=cb[:, kc:kc + 1],
                                    start=(kc == 0), stop=(kc == KC - 1)),
                                    r=[wb_, B("cb")], acc=pb, w=[pb], inc=(kc == KC - 1))
                    S.op("dve", lambda e, v=v, pt=pt: e.tensor_tensor(out=mod[:, v, :], in0=pt[:, 0:KC], in1=bad[:, v, :], op=ALU.add),
                         r=[pb, B("bad")], w=[B("mod")])
                for i, (nwi, sci, shi) in enumerate(((0, 1, 0), (1, 4, 3), (2, 7, 6))):
                    S.op("dve", lambda e, i=i, nwi=nwi, sci=sci: e.scalar_tensor_tensor(
                        out=AB[:, 2 * i, :], in0=mod[:, sci, :], scalar=1.0, in1=vec[:, nwi, :], op0=ALU.add, op1=ALU.mult),
                        r=[B("mod"), B("vec")], w=[B("AB")])
                    S.op("dve", lambda e, i=i, shi=shi: e.tensor_copy(out=AB[:, 2 * i + 1, :], in_=mod[:, shi, :]),
                         r=[B("mod")], w=[B("AB")])
            S.barrier()
        if "mod" in dbg:
            S.dma("sp", dbg["mod"], mod[:], r=[B("mod")])
            S.dma("sp", dbg["AB"], AB[:], r=[B("AB")])

        O_SBQ, O_SBK, O_SBV, O_MLQ, O_MLK, O_MLV, O_MLO, O_IF = 0, 2048, 4096, 6144, 8192, 10240, 12288, 14336
        if on("p1"):
            with ExitStack() as ps_:
                sbp = lambda name, shape, dtype=F32: ps_.enter_context(nc.sbuf_tensor(name, list(shape), dtype))
                PS = [ps_.enter_context(nc.psum_tensor("p1ps%d" % i, [128, 512], F32)) for i in range(7)]
                PSB = ps_.enter_context(nc.psum_tensor("p1psb", [128, 512], BF16))
                xh = [sbp("xh%d" % i, [128, 16, SL]) for i in range(2)]
                uT = sbp("uT", [128, KC, 2 * SL], BF16)
                W = [sbp("W%d" % i, [128, KC, 256], BF16) for i in range(3)]
                Wif = sbp("Wif", [128, KC, 8], BF16)
                sq = [sbp("sq%d" % i, [128, SL], BF16) for i in range(3)]
                tmp = [sbp("tmp%d" % i, [128, SL]) for i in range(3)]
                rstd = sbp("rstd", [128, SL])
                ABs = sbp("ABs", [128, 2, KC])
                ev = [sbp("ev%d" % i, [128, SL], BF16) for i in range(4)]
                evf = [sbp("evf%d" % i, [128, 8]) for i in range(2)]
                zt = sbp("zt", [128, 8], BF16)
                S.op("dve", lambda e: e.memset(zt[:], 0.0), w=[B("zt")])
                for c in range(16):
                    S.dma("sp", mlk_scr[c * 128:(c + 1) * 128, 0:8], zt[:], r=[B("zt")], w=[B("mlk_scr", "halo")])
                S.dma("pool", Wif[:], w_in[:, O_IF:O_IF + 8].rearrange("(c p) n -> p c n", p=128), w=[B("Wif")])
                wi = [0]
                evi = [0]
                psi = [0]

                def loadW(c0):
                    k = wi[0] % 3
                    wi[0] += 1
                    S.dma("pool", W[k][:], w_in[:, c0:c0 + 256].rearrange("(c p) n -> p c n", p=128), w=[B("W", k)])
                    return W[k], B("W", k)

                def nextps():
                    k = psi[0] % 6
                    psi[0] += 1
                    return PS[k], B("ps", k)

                def evac(pt, pb, n, dst, dstbufs, use_act):
                    k = evi[0] % 4
                    evi[0] += 1
                    et, eb = ev[k], B("ev", k)
                    if use_act:
                        S.op("act", lambda e: e.activation(out=et[:, 0:n], in_=pt[:, 0:n], func=AF.Copy), r=[pb], w=[eb])
                    else:
                        S.op("dve", lambda e: e.tensor_copy(out=et[:, 0:n], in_=pt[:, 0:n]), r=[pb], w=[eb])
                    S.dma("sp", dst, et[:, 0:n], r=[eb], w=dstbufs)

                def feat_major(c0, dst_rows, tok_lo, ntok, dst, dstbufs):
                    Wt, Wb = loadW(c0)
                    for cc in range(2):
                        pt, pb = nextps()
                        for kc in range(KC):
                            S.op("pe", lambda e, kc=kc, cc=cc, pt=pt, Wt=Wt: e.matmul(
                                pt[:, 0:ntok], lhsT=Wt[:, kc, cc * 128:(cc + 1) * 128], rhs=uT[:, kc, tok_lo:tok_lo + ntok],
                                start=(kc == 0), stop=(kc == KC - 1)),
                                r=[Wb, B("uT")], acc=pb, w=[pb], inc=(kc == KC - 1))
                        evac(pt, pb, ntok, dst(cc), dstbufs, use_act=(cc == 0))

                def tok_major(c0, tblocks, dst, dstbufs):
                    Wt, Wb = loadW(c0)
                    for i, tb in enumerate(tblocks):
                        pt, pb = nextps()
                        for kc in range(KC):
                            S.op("pe", lambda e, kc=kc, tb=tb, pt=pt, Wt=Wt: e.matmul(
                                pt[:, 0:256], lhsT=uT[:, kc, tb * 128:(tb + 1) * 128], rhs=Wt[:, kc, :],
                                start=(kc == 0), stop=(kc == KC - 1)),
                                r=[Wb, B("uT")], acc=pb, w=[pb], inc=(kc == KC - 1))
                        evac(pt, pb, 256, dst(tb), dstbufs, use_act=(i % 2 == 0))

                for T in range(NSLOT // 2):
                    for half in range(2):
                        s = 2 * T + half
                        tcol = half * SL
                        S.op("dve", lambda e, s=s: e.tensor_scalar(out=ABs[:, 0, :], in0=AB[:, 0, :], scalar1=sval[:, s:s + 1], scalar2=None, op0=ALU.mult),
                             r=[B("AB"), B("sval")], w=[B("ABs")])
                        S.op("dve", lambda e, s=s: e.tensor_scalar(out=ABs[:, 1, :], in0=AB[:, 1, :], scalar1=sval[:, s:s + 1], scalar2=None, op0=ALU.mult),
                             r=[B("AB"), B("sval")], w=[B("ABs")])
                        pt, pb = PS[6], B("ps", 6)
                        for pas in range(2):
                            for hh in range(2):
                                xt, xb = xh[hh], B("xh", hh)
                                S.dma("act" if hh else "sp", xt[:], xs[hh * 2048:(hh + 1) * 2048, s * SL:(s + 1) * SL].rearrange("(c p) t -> p c t", p=128), w=[xb])
                                for k16 in range(16):
                                    kc = hh * 16 + k16
                                    if pas == 0:
                                        qi = kc % 3
                                        S.op("act", lambda e, qi=qi, xt=xt, k16=k16: e.activation(out=sq[qi][:], in_=xt[:, k16, :], func=AF.Square),
                                             r=[xb], w=[B("sq", qi)])
                                        S.op("pe", lambda e, qi=qi, kc=kc: e.matmul(pt[:], lhsT=cm_b[:, ONE, :], rhs=sq[qi][:], start=(kc == 0), stop=(kc == KC - 1)),
                                             r=[B("sq", qi), B("cm_b")], acc=pb, w=[pb], inc=True)
                                    else:
                                        ti = kc % 3
                                        S.op("dve", lambda e, ti=ti, xt=xt, k16=k16: e.tensor_tensor(out=tmp[ti][:], in0=xt[:, k16, :], in1=rstd[:], op=ALU.mult),
                                             r=[xb, B("rstd")], w=[B("tmp", ti)])
                                        S.op("act", lambda e, ti=ti, kc=kc, tcol=tcol: e.activation(
                                            out=uT[:, kc, tcol:tcol + SL], in_=tmp[ti][:], func=AF.Identity,
                                            scale=ABs[:, 0, kc:kc + 1], bias=ABs[:, 1, kc:kc + 1]),
                                            r=[B("tmp", ti), B("ABs")], w=[B("uT")])
                            if pas == 0:
                                S.op("dve", lambda e: e.tensor_scalar(out=rstd[:], in0=pt[:], scalar1=1.0 / D, scalar2=EPS, op0=ALU.mult, op1=ALU.add),
                                     r=[pb], w=[B("rstd")])
                                S.op("act", lambda e: e.activation(out=rstd[:], in_=rstd[:], func=AF.Sqrt), r=[B("rstd")], w=[B("rstd")])
                                S.op("dve", lambda e: e.reciprocal(out=rstd[:], in_=rstd[:]), r=[B("rstd")], w=[B("rstd")])
                    t0 = 2 * T * SL
                    for (obase, scr, off, scrn) in ((O_SBK, kT_scr, 0, 'kT_scr'), (O_MLK, mlk_scr, 8, 'mlk_scr')):
                        for g in range(8):
                            Wt, Wb = loadW(obase + g * 256)
                            for half in range(2):
                                for cc in range(2):
                                    pt, pb = nextps()
                                    for kc in range(KC):
                                        S.op("pe", lambda e, kc=kc, cc=cc, pt=pt, Wt=Wt, half=half: e.matmul(
                                            pt[:], lhsT=Wt[:, kc, cc * 128:(cc + 1) * 128], rhs=uT[:, kc, half * SL:(half + 1) * SL],
                                            start=(kc == 0), stop=(kc == KC - 1)),
                                            r=[Wb, B("uT")], acc=pb, w=[pb], inc=(kc == KC - 1))
                                    r0 = g * 256 + cc * 128
                                    evac(pt, pb, SL, scr[r0:r0 + 128, off + t0 + half * SL: off + t0 + (half + 1) * SL], [B(scrn)], use_act=(cc == 0))
                    for g in range(8):
                        Wt, Wb = loadW(O_SBV + g * 256)
                        for tb in range(8):
                            pt, pb = nextps()
                            for kc in range(KC):
                                S.op("pe", lambda e, kc=kc, tb=tb, pt=pt, Wt=Wt: e.matmul(
                                    pt[:, 0:256], lhsT=uT[:, kc, tb * 128:(tb + 1) * 128], rhs=Wt[:, kc, :],
                                    start=(kc == 0), stop=(kc == KC - 1)),
                                    r=[Wb, B("uT")], acc=pb, w=[pb], inc=(kc == KC - 1))
                            blk = 2 * T * 4 + tb
                            k = evi[0] % 4
                            evi[0] += 1
                            et, eb = ev[k], B("ev", k)
                            if tb % 2:
                                S.op("act", lambda e, et=et, pt=pt: e.activation(out=et[:, 0:256], in_=pt[:, 0:256], func=AF.Copy), r=[pb], w=[eb])
                            else:
                                S.op("dve", lambda e, et=et, pt=pt: e.tensor_copy(out=et[:, 0:256], in_=pt[:, 0:256]), r=[pb], w=[eb])
                            for hh in range(2):
                                S.dma("sp", v_scr[2 * g + hh, :, blk, :], et[:, hh * 128:(hh + 1) * 128], r=[eb], w=[B("v_scr")])
                    for g in range(8):
                        Wt, Wb = loadW(O_MLV + g * 256)
                        for tb in range(8):
                            pt, pb = nextps()
                            for kc in range(KC):
                                S.op("pe", lambda e, kc=kc, tb=tb, pt=pt, Wt=Wt: e.matmul(
                                    pt[:, 0:256], lhsT=uT[:, kc, tb * 128:(tb + 1) * 128], rhs=Wt[:, kc, :],
                                    start=(kc == 0), stop=(kc == KC - 1)),
                                    r=[Wb, B("uT")], acc=pb, w=[pb], inc=(kc == KC - 1))
                            blk = 2 * T * 4 + tb
                            evac(pt, pb, 256, mlv_scr[g // 2, :, blk, (g % 2) * 256:(g % 2 + 1) * 256], [B("mlv_scr")], use_act=(tb % 2 == 1))
                    for tb in range(8):
                        pt, pb = nextps()
                        for kc in range(KC):
                            S.op("pe", lambda e, kc=kc, tb=tb, pt=pt: e.matmul(
                                pt[:, 0:8], lhsT=uT[:, kc, tb * 128:(tb + 1) * 128], rhs=Wif[:, kc, :],
                                start=(kc == 0), stop=(kc == KC - 1)),
                                r=[B("Wif"), B("uT")], acc=pb, w=[pb], inc=(kc == KC - 1))
                        k = tb % 2
                        S.op("dve", lambda e, k=k, pt=pt: e.tensor_copy(out=evf[k][:], in_=pt[:, 0:8]), r=[pb], w=[B("evf", k)])
                        S.dma("sp", if_scr[:, 2 * T * 4 + tb, :], evf[k][:], r=[B("evf", k)], w=[B("if_scr")])
                    if (2 * T + 1) in OWN:
                        oi = OWN.index(2 * T + 1)
                        for g in range(8):
                            Wt, Wb = loadW(O_SBQ + g * 256)
                            for cc in range(2):
                                pt, pb = nextps()
                                for kc in range(KC):
                                    S.op("pe", lambda e, kc=kc, cc=cc, pt=pt, Wt=Wt: e.matmul(
                                        pt[:], lhsT=Wt[:, kc, cc * 128:(cc + 1) * 128], rhs=uT[:, kc, SL:2 * SL],
                                        start=(kc == 0), stop=(kc == KC - 1)),
                                        r=[Wb, B("uT")], acc=pb, w=[pb], inc=(kc == KC - 1))
                                r0 = g * 256 + cc * 128
                                evac(pt, pb, SL, qT_scr[r0:r0 + 128, oi * SL:(oi + 1) * SL], [B("qT_scr")], use_act=(cc == 0))
                        for g in range(8):
                            Wt, Wb = loadW(O_MLQ + g * 256)
                            for cc in range(2):
                                r0 = g * 256 + cc * 128
                                for (lo, n, dcol) in ((SL, SL, 8), (SL - 8, 8, 0)):
                                    pt, pb = nextps()
                                    for kc in range(KC):
                                        S.op("pe", lambda e, kc=kc, cc=cc, pt=pt, Wt=Wt, lo=lo, n=n: e.matmul(
                                            pt[:, 0:n], lhsT=Wt[:, kc, cc * 128:(cc + 1) * 128], rhs=uT[:, kc, lo:lo + n],
                                            start=(kc == 0), stop=(kc == KC - 1)),
                                            r=[Wb, B("uT")], acc=pb, w=[pb], inc=(kc == KC - 1))
                                    evac(pt, pb, n, mlq_scr[r0:r0 + 128, oi * 520 + dcol: oi * 520 + dcol + n], [B("mlq_scr")], use_act=(cc == 0))
                        for g in range(8):
                            Wt, Wb = loadW(O_MLO + g * 256)
                            for tb in range(4, 8):
                                pt, pb = nextps()
                                for kc in range(KC):
                                    S.op("pe", lambda e, kc=kc, tb=tb, pt=pt, Wt=Wt: e.matmul(
                                        pt[:, 0:256], lhsT=uT[:, kc, tb * 128:(tb + 1) * 128], rhs=Wt[:, kc, :],
                                        start=(kc == 0), stop=(kc == KC - 1)),
                                        r=[Wb, B("uT")], acc=pb, w=[pb], inc=(kc == KC - 1))
                                evac(pt, pb, 256, mlo_scr[g // 2, :, oi * 4 + tb - 4, (g % 2) * 256:(g % 2 + 1) * 256], [B("mlo_scr")], use_act=(tb % 2 == 1))
            S.barrier()
        if on("p2"):
            with ExitStack() as ps_:
                sbp = lambda name, shape, dtype=F32: ps_.enter_context(nc.sbuf_tensor(name, list(shape), dtype))
                PS = [ps_.enter_context(nc.psum_tensor("p2ps%d" % i, [128, 512], F32)) for i in range(8)]
                kTt = [sbp("kTt%d" % i, [128, NT], BF16) for i in range(2)]
                vt = [sbp("vt%d" % i, [128, 64, 128], BF16) for i in range(2)]
                qt = [sbp("qt%d" % i, [128, 4 * SL], BF16) for i in range(2)]
                msk = sbp("msk", [128, 4, SL])
                e_t = [[sbp("e_t%d_%d" % (s_, i), [128, SL]) for i in range(2)] for s_ in range(2)]
                a_t = [[sbp("a_t%d_%d" % (s_, i), [128, SL]) for i in range(2)] for s_ in range(2)]
                spf = [sbp("spf%d" % s_, [128, SL]) for s_ in range(2)]
                spb = [[sbp("spb%d_%d" % (s_, i), [128, SL], BF16) for i in range(2)] for s_ in range(2)]
                E_t = [[sbp("E_t%d_%d" % (s_, i), [128, SL]) for i in range(2)] for s_ in range(2)]
                wf = [sbp("wf%d" % s_, [128, SL]) for s_ in range(2)]
                wb = [[sbp("wb%d_%d" % (s_, i), [128, SL], BF16) for i in range(2)] for s_ in range(2)]
                ob = [sbp("ob%d" % s_, [128, SL], BF16) for s_ in range(2)]
                Rc = [sbp("Rc%d" % s_, [128, SL]) for s_ in range(2)]
                S.dma("sp", msk[:], cmask[:], w=[B("msk")])
                if not on("p1") and not _os.environ.get("P2NOINIT"):
                    S.op("dve", lambda e: e.memset(kTt[0][:], 0.01), w=[B("kTt", 0)])
                    for h_ in range(16):
                        S.dma("sp", kT_scr[h_ * 128:(h_ + 1) * 128, :], kTt[0][:], r=[B("kTt", 0)], w=[B("kT_scr")])
                        S.dma("sp", qT_scr[h_ * 128:(h_ + 1) * 128, :], kTt[0][:, 0:2048], r=[B("kTt", 0)], w=[B("qT_scr")])
                        S.dma("sp", v_scr[h_], kTt[0][:].rearrange("p (a b) -> p a b", b=128), r=[B("kTt", 0)], w=[B("v_scr")])
                scale = float(1.0 / np.sqrt(128.0))
                nh = int(_os.environ.get('P2H', '16'))
                for hp in range(nh // 2):
                    for s_ in range(2):
                        h = 2 * hp + s_
                        S.dma("sp", kTt[s_][:], kT_scr[h * 128:(h + 1) * 128, :], r=[B("kT_scr")], w=[B("kTt", s_)])
                        S.dma("act", vt[s_][:], v_scr[h], r=[B("v_scr")], w=[B("vt", s_)])
                        S.dma("sp", qt[s_][:], qT_scr[h * 128:(h + 1) * 128, :], r=[B("qT_scr")], w=[B("qt", s_)])
                    for oi, so in list(enumerate(OWN))[:int(_os.environ.get('P2O', '4'))]:
                        nblk = (so + 1) * 4

                        def stage1(s_, bi):
                            jb = nblk - 1 - bi
                            m = jb - so * 4
                            i2 = bi % 2
                            zp, zb = PS[4 * s_], B("ps", 4 * s_)
                            S.op("pe", lambda e: e.matmul(zp[:], lhsT=kTt[s_][:, jb * 128:(jb + 1) * 128], rhs=qt[s_][:, oi * SL:(oi + 1) * SL], start=True, stop=True),
                                 r=[B("kTt", s_), B("qt", s_)], w=[zb])
                            S.op("act", lambda e: e.activation(out=e_t[s_][i2][:], in_=zp[:], func=AF.Exp, scale=scale),
                                 r=[zb], w=[B("e_t", s_, i2), B("zser", s_)])
                            if bi == 0:
                                S.op("dve", lambda e: e.tensor_scalar(out=a_t[s_][i2][:], in0=zp[:], scalar1=scale, scalar2=None, op0=ALU.mult),
                                     r=[zb, B("zser", s_)], w=[B("a_t", s_, i2)])
                            else:
                                S.op("dve", lambda e: e.scalar_tensor_tensor(out=a_t[s_][i2][:], in0=zp[:], scalar=scale, in1=Rc[s_][:], op0=ALU.mult, op1=ALU.subtract),
                                     r=[zb, B("Rc", s_), B("zser", s_)], w=[B("a_t", s_, i2)])
                            if m >= 0:
                                S.op("act", lambda e: e.activation(out=spf[s_][:], in_=e_t[s_][i2][:], func=AF.Ln, bias=1.0),
                                     r=[B("e_t", s_, i2)], w=[B("spf", s_)])
                                S.op("pool", lambda e: e.tensor_tensor(out=spb[s_][i2][:], in0=spf[s_][:], in1=msk[:, m, :], op=ALU.mult),
                                     r=[B("spf", s_), B("msk")], w=[B("spb", s_, i2)])
                            else:
                                S.op("act", lambda e: e.activation(out=spb[s_][i2][:], in_=e_t[s_][i2][:], func=AF.Ln, bias=1.0),
                                     r=[B("e_t", s_, i2)], w=[B("spb", s_, i2)])

                        def stage2(s_, bi):
                            jb = nblk - 1 - bi
                            m = jb - so * 4
                            i2 = bi % 2
                            xp, xb = PS[4 * s_ + 1], B("ps", 4 * s_ + 1)
                            yp, yb = PS[4 * s_ + 2], B("ps", 4 * s_ + 2)
                            S.op("pe", lambda e: e.matmul(xp[:], lhsT=cm_b[:, UTm, :], rhs=spb[s_][i2][:], start=True, stop=True),
                                 r=[B("spb", s_, i2), B("cm_b")], w=[xb])
                            if bi < nblk - 1:
                                S.op("pe", lambda e: e.matmul(yp[:], lhsT=cm_b[:, ONE, :], rhs=spb[s_][i2][:], start=True, stop=True),
                                     r=[B("spb", s_, i2), B("cm_b")], w=[yb])
                            S.op("dve", lambda e: e.tensor_tensor(out=E_t[s_][i2][:], in0=a_t[s_][i2][:], in1=xp[:], op=ALU.subtract),
                                 r=[B("a_t", s_, i2), xb], w=[B("E_t", s_, i2)])
                            if bi < nblk - 1:
                                if bi == 0:
                                    S.op("dve", lambda e: e.tensor_copy(out=Rc[s_][:], in_=yp[:]), r=[yb], w=[B("Rc", s_)])
                                else:
                                    S.op("dve", lambda e: e.tensor_tensor(out=Rc[s_][:], in0=Rc[s_][:], in1=yp[:], op=ALU.add), r=[yb, B("Rc", s_)], w=[B("Rc", s_)])
                            if m >= 0:
                                S.op("act", lambda e: e.activation(out=wf[s_][:], in_=E_t[s_][i2][:], func=AF.Exp),
                                     r=[B("E_t", s_, i2)], w=[B("wf", s_)])
                                S.op("pool", lambda e: e.tensor_tensor(out=wb[s_][i2][:], in0=wf[s_][:], in1=msk[:, m, :], op=ALU.mult),
                                     r=[B("wf", s_), B("msk")], w=[B("wb", s_, i2)])
                            else:
                                S.op("act", lambda e: e.activation(out=wb[s_][i2][:], in_=E_t[s_][i2][:], func=AF.Exp),
                                     r=[B("E_t", s_, i2)], w=[B("wb", s_, i2)])

                        def stage3(s_, bi):
                            jb = nblk - 1 - bi
                            i2 = bi % 2
                            outp, outb = PS[4 * s_ + 3], B("ps", 4 * s_ + 3)
                            S.op("pe", lambda e: e.matmul(outp[:], lhsT=vt[s_][:, jb, :], rhs=wb[s_][i2][:], start=(bi == 0), stop=(bi == nblk - 1)),
                                 r=[B("wb", s_, i2), B("vt", s_)], acc=outb, w=[outb], inc=True)

                        for bi in range(nblk + 1):
                            if bi < nblk:
                                stage1(0, bi)
                                stage1(1, bi)
                            if bi >= 1:
                                stage3(0, bi - 1)
                                stage3(1, bi - 1)
                            if bi < nblk:
                                stage2(0, bi)
                                stage2(1, bi)
                        for s_ in range(2):
                            h = 2 * hp + s_
                            outp, outb = PS[4 * s_ + 3], B("ps", 4 * s_ + 3)
                            S.op("act", lambda e, s_=s_, outp=outp: e.activation(out=ob[s_][:], in_=outp[:], func=AF.Copy), r=[outb], w=[B("ob", s_)])
                            S.dma("sp", yT_scr[h * 128:(h + 1) * 128, oi * SL:(oi + 1) * SL], ob[s_][:], r=[B("ob", s_)], w=[B("yT_scr")])
            S.barrier()
        if on("p3"):
            with ExitStack() as ps_:
                sbp = lambda name, shape, dtype=F32: ps_.enter_context(nc.sbuf_tensor(name, list(shape), dtype))
                PS = [ps_.enter_context(nc.psum_tensor("p3ps%d" % i, [128, 512], F32)) for i in range(7)]
                PSB = ps_.enter_context(nc.psum_tensor("p3psb", [128, 512], BF16))
                cw = sbp("cw", [128, 4, KC])
                gb = sbp("gb", [128, 8])
                nwb = sbp("nwb", [128, 2048])
                iff = sbp("iff", [128, 64, 8])
                lf = sbp("lf", [128, 64])
                ii = sbp("ii", [128, 64])
                bcum = sbp("bcum", [128, 64])
                bL = sbp("bL", [128, 64])
                gA = sbp("gA", [128, 64])
                gK = sbp("gK", [128, 64])
                dec = sbp("dec", [128, 64])
                ebi = sbp("ebi", [128, 64])
                t64 = sbp("t64", [128, 64])
                kpre = sbp("kpre", [128, 4, SL + 8], BF16)
                kcv = sbp("kcv", [128, 4, SL])
                kTc = sbp("kTc", [128, 4, SL], BF16)
                qpre = sbp("qpre", [128, 4, SL + 8], BF16)
                qTc = sbp("qTc", [128, 4, SL], BF16)
                vch = sbp("vch", [128, 4, 512], BF16)
                och = sbp("och", [128, 4, 512], BF16)
                kw = [sbp("kw%d" % i, [128, 512], BF16) for i in range(2)]
                Cst = sbp("Cst", [128, 4, 512])
                Cbf = sbp("Cbf", [128, 4, 512], BF16)
                nst = sbp("nst", [128, 4])
                nbf = sbp("nbf", [128, 4], BF16)
                Ab = sbp("Ab", [128, 128], BF16)
                hm = sbp("hm", [128, 512])
                sg = sbp("sg", [128, 512])
                y1 = sbp("y1", [128, 512])
                y2 = sbp("y2", [128, 512], BF16)
                yTb = sbp("yTb", [128, 512], BF16)
                sm = sbp("sm", [128, 8])
                S.dma("sp", cw[:], convw[:], w=[B("cw")])
                S.dma("sp", gb[:], gbias[:], w=[B("gb")])
                S.dma("sp", nwb[:], mlnw[:], w=[B("nwb")])
                S.dma("sp", iff[:], if_scr, r=[B("if_scr")], w=[B("iff")])
                isq = 1.0 / np.sqrt(512.0)

                def conv_silu(pre, preb, out_bf, outb, chbase, hd):
                    for dc in range(4):
                        ch = chbase + hd * 4 + dc
                        S.op("dve", lambda e, dc=dc, ch=ch: e.tensor_scalar(out=kcv[:, dc, :], in0=pre[:, dc, 5:5 + SL], scalar1=cw[:, 0, ch:ch + 1], scalar2=None, op0=ALU.mult),
                             r=[preb, B("cw")], w=[B("kcv")])
                        for i in range(1, 4):
                            S.op("dve", lambda e, dc=dc, ch=ch, i=i: e.scalar_tensor_tensor(
                                out=kcv[:, dc, :], in0=pre[:, dc, 5 + i:5 + i + SL], scalar=cw[:, i, ch:ch + 1], in1=kcv[:, dc, :], op0=ALU.mult, op1=ALU.add),
                                r=[preb, B("cw"), B("kcv")], w=[B("kcv")])
                        S.op("act", lambda e, dc=dc: e.activation(out=out_bf[:, dc, :], in_=kcv[:, dc, :], func=AF.Silu), r=[B("kcv")], w=[outb])

                for hd in range(4):
                    S.op("dve", lambda e, hd=hd: e.tensor_scalar(out=ii[:], in0=iff[:, :, hd], scalar1=gb[:, hd:hd + 1], scalar2=None, op0=ALU.add),
                         r=[B("iff"), B("gb")], w=[B("ii")])
                    S.op("dve", lambda e, hd=hd: e.tensor_scalar(out=t64[:], in0=iff[:, :, 4 + hd], scalar1=gb[:, 4 + hd:5 + hd], scalar2=None, op0=ALU.add),
                         r=[B("iff"), B("gb")], w=[B("t64")])
                    S.op("act", lambda e: e.activation(out=t64[:], in_=t64[:], func=AF.Exp, scale=-1.0), r=[B("t64")], w=[B("t64")])
                    S.op("act", lambda e: e.activation(out=t64[:], in_=t64[:], func=AF.Ln, bias=1.0), r=[B("t64")], w=[B("t64")])
                    S.op("dve", lambda e: e.tensor_scalar(out=lf[:], in0=t64[:], scalar1=-1.0, scalar2=None, op0=ALU.mult), r=[B("t64")], w=[B("lf")])
                    p0, p0b = PS[0], B("ps", 0)
                    S.op("pe", lambda e: e.matmul(p0[:, 0:64], lhsT=cm_f[:, LEm, :], rhs=lf[:], start=True, stop=True), r=[B("cm_f"), B("lf")], w=[p0b])
                    S.op("pe", lambda e: e.matmul(p0[:, 64:128], lhsT=cm_f[:, ONE, :], rhs=lf[:], start=True, stop=True), r=[B("cm_f"), B("lf")], w=[p0b])
                    S.op("dve", lambda e: e.tensor_copy(out=bcum[:], in_=p0[:, 0:64]), r=[p0b], w=[B("bcum")])
                    S.op("dve", lambda e: e.tensor_copy(out=bL[:], in_=p0[:, 64:128]), r=[p0b], w=[B("bL")])
                    S.op("act", lambda e: e.activation(out=dec[:], in_=bL[:], func=AF.Exp), r=[B("bL")], w=[B("dec")])
                    S.op("act", lambda e: e.activation(out=ebi[:], in_=bcum[:], func=AF.Exp, scale=-1.0), r=[B("bcum")], w=[B("ebi")])
                    S.op("dve", lambda e: e.tensor_tensor(out=t64[:], in0=ii[:], in1=bcum[:], op=ALU.subtract), r=[B("ii"), B("bcum"), B("lf")], w=[B("t64")])
                    S.op("act", lambda e: e.activation(out=gA[:], in_=t64[:], func=AF.Exp), r=[B("t64")], w=[B("gA")])
                    S.op("dve", lambda e: e.tensor_scalar(out=gA[:], in0=gA[:], scalar1=float(isq), scalar2=None, op0=ALU.mult), r=[B("gA")], w=[B("gA")])
                    S.op("dve", lambda e: e.tensor_tensor(out=gK[:], in0=gA[:], in1=dec[:], op=ALU.mult), r=[B("gA"), B("dec")], w=[B("gK")])
                    S.op("dve", lambda e: e.memset(Cst[:], 0.0), w=[B("Cst", d_) for d_ in range(4)])
                    S.op("pool", lambda e: e.memset(Cbf[:], 0.0), w=[B("Cbf")])
                    S.op("dve", lambda e: e.memset(nst[:], 0.0), w=[B("nst")])
                    S.op("pool", lambda e: e.memset(nbf[:], 0.0), w=[B("nbf")])
                    for s in range(NSLOT):
                        own = s in OWN
                        S.dma("sp", kpre[:], mlk_scr[hd * 512:(hd + 1) * 512, s * SL: s * SL + SL + 8].rearrange("(c p) t -> p c t", p=128),
                              r=[B("mlk_scr"), B("mlk_scr", "halo")], w=[B("kpre")])
                        conv_silu(kpre, B("kpre"), kTc, B("kTc"), 16, hd)
                        if own:
                            oi = OWN.index(s)
                            S.dma("act", qpre[:], mlq_scr[hd * 512:(hd + 1) * 512, oi * 520: oi * 520 + 520].rearrange("(c p) t -> p c t", p=128),
                                  r=[B("mlq_scr")], w=[B("qpre")])
                            conv_silu(qpre, B("qpre"), qTc, B("qTc"), 0, hd)
                            S.dma("act", och[:], mlo_scr[hd, :, oi * 4:(oi + 1) * 4, :], r=[B("mlo_scr")], w=[B("och")])
                        S.dma("sp", vch[:], mlv_scr[hd, :, s * 4:(s + 1) * 4, :], r=[B("mlv_scr")], w=[B("vch")])
                        for c4 in range(4):
                            c = s * 4 + c4
                            tsl = slice(c4 * 128, (c4 + 1) * 128)
                            for dc in range(4):
                                S.op("pe", lambda e, dc=dc, tsl=tsl: e.transpose(PSB[:, dc * 128:(dc + 1) * 128], kTc[:, dc, tsl], cm_b[:, IDF, :]),
                                     r=[B("kTc"), B("cm_b")], w=[B("psb")])
                            kwi = c % 2
                            S.op("dve", lambda e, kwi=kwi, c=c: e.tensor_scalar(out=kw[kwi][:], in0=PSB[:], scalar1=gK[:, c:c + 1], scalar2=None, op0=ALU.mult),
                                 r=[B("psb"), B("gK")], w=[B("kw", kwi)])
                            if own:
                                p1, p1b = PS[1], B("ps", 1)
                                for dc in range(4):
                                    S.op("pe", lambda e, dc=dc, tsl=tsl: e.matmul(p1[:, 0:128], lhsT=kTc[:, dc, tsl], rhs=qTc[:, dc, tsl], start=(dc == 0), stop=(dc == 3)),
                                         r=[B("kTc"), B("qTc")], acc=p1b, w=[p1b], inc=(dc == 3))
                                S.op("dve", lambda e, c=c: e.scalar_tensor_tensor(out=Ab[:], in0=p1[:, 0:128], scalar=gA[:, c:c + 1], in1=cm_f[:, LEm, :], op0=ALU.mult, op1=ALU.mult),
                                     r=[p1b, B("gA"), B("cm_f")], w=[B("Ab")])
                                p2, p2b = PS[2], B("ps", 2)
                                for dc in range(4):
                                    S.op("pe", lambda e, dc=dc, tsl=tsl: e.matmul(p2[:], lhsT=qTc[:, dc, tsl], rhs=Cbf[:, dc, :], start=(dc == 0), stop=False),
                                         r=[B("qTc"), B("Cbf")], acc=p2b, w=[p2b], inc=False)
                                S.op("pe", lambda e, c4=c4: e.matmul(p2[:], lhsT=Ab[:], rhs=vch[:, c4, :], start=False, stop=True),
                                     r=[B("Ab"), B("vch")], acc=p2b, w=[p2b])
                                p3, p3b = PS[3], B("ps", 3)
                                for dc in range(4):
                                    S.op("pe", lambda e, dc=dc, tsl=tsl: e.matmul(p3[:, 0:1], lhsT=qTc[:, dc, tsl], rhs=nbf[:, dc:dc + 1], start=(dc == 0), stop=False),
                                         r=[B("qTc"), B("nbf")], acc=p3b, w=[p3b], inc=False)
                                S.op("pe", lambda e: e.matmul(p3[:, 0:1], lhsT=Ab[:], rhs=onescol_b[:], start=False, stop=True),
                                     r=[B("Ab"), B("onescol")], acc=p3b, w=[p3b])
                                S.op("act", lambda e: e.activation(out=sm[:, 0:1], in_=p3[:, 0:1], func=AF.Abs), r=[p3b], w=[B("sm")])
                                S.op("dve", lambda e, c=c: e.tensor_tensor(out=sm[:, 1:2], in0=sm[:, 0:1], in1=ebi[:, c:c + 1], op=ALU.max), r=[B("sm"), B("ebi")], w=[B("sm")])
                                S.op("dve", lambda e: e.reciprocal(out=sm[:, 2:3], in_=sm[:, 1:2]), r=[B("sm")], w=[B("sm")])
                                S.op("act", lambda e: e.activation(out=hm[:], in_=p2[:], func=AF.Copy, scale=sm[:, 2:3]), r=[p2b, B("sm")], w=[B("hm")])
                                S.op("dve", lambda e: e.memset(sm[:, 3:4], 0.0), r=[B("sm")], w=[B("sm")])
                                S.op("act", lambda e: e.activation(out=sg[:], in_=hm[:], func=AF.Square, accum_out=sm[:, 3:4]), r=[B("hm"), B("sm")], w=[B("sg"), B("sm")])
                                S.op("dve", lambda e: e.tensor_scalar(out=sm[:, 4:5], in0=sm[:, 3:4], scalar1=1.0 / 512.0, scalar2=EPS, op0=ALU.mult, op1=ALU.add), r=[B("sm")], w=[B("sm")])
                                S.op("act", lambda e: e.activation(out=sm[:, 5:6], in_=sm[:, 4:5], func=AF.Sqrt), r=[B("sm")], w=[B("sm")])
                                S.op("dve", lambda e: e.reciprocal(out=sm[:, 6:7], in_=sm[:, 5:6]), r=[B("sm")], w=[B("sm")])
                                S.op("act", lambda e, c4=c4: e.activation(out=sg[:], in_=och[:, c4, :], func=AF.Sigmoid), r=[B("och"), B("sg")], w=[B("sg")])
                                S.op("dve", lambda e, hd=hd: e.scalar_tensor_tensor(out=y1[:], in0=hm[:], scalar=sm[:, 6:7], in1=nwb[:, hd * 512:(hd + 1) * 512], op0=ALU.mult, op1=ALU.mult),
                                     r=[B("hm"), B("sm"), B("nwb")], w=[B("y1")])
                                S.op("pool", lambda e: e.tensor_tensor(out=y2[:], in0=y1[:], in1=sg[:], op=ALU.mult), r=[B("y1"), B("sg")], w=[B("y2")])
                                for ec in range(4):
                                    S.op("pe", lambda e, ec=ec: e.transpose(PSB[:, ec * 128:(ec + 1) * 128], y2[:, ec * 128:(ec + 1) * 128], cm_b[:, IDF, :]),
                                         r=[B("y2"), B("cm_b")], w=[B("psb")])
                                S.op("act", lambda e: e.activation(out=yTb[:], in_=PSB[:], func=AF.Copy), r=[B("psb")], w=[B("yTb")])
                                oi = OWN.index(s)
                                for ec in range(4):
                                    r0 = 2048 + hd * 512 + ec * 128
                                    S.dma("sp", yT_scr[r0:r0 + 128, oi * SL + c4 * 128: oi * SL + (c4 + 1) * 128], yTb[:, ec * 128:(ec + 1) * 128],
                                          r=[B("yTb")], w=[B("yT_scr")])
                            p4, p4b = PS[6], B("ps", 6)
                            for dc in range(4):
                                S.op("pe", lambda e, dc=dc, kwi=kwi: e.matmul(p4[:, dc:dc + 1], lhsT=kw[kwi][:, dc * 128:(dc + 1) * 128], rhs=onescol_b[:], start=True, stop=True),
                                     r=[B("kw", kwi), B("onescol")], w=[p4b])
                            S.op("dve", lambda e, c=c: e.scalar_tensor_tensor(out=nst[:], in0=nst[:], scalar=dec[:, c:c + 1], in1=p4[:, 0:4], op0=ALU.mult, op1=ALU.add),
                                 r=[B("nst"), B("dec"), p4b], w=[B("nst")])
                            S.op("pool", lambda e: e.tensor_copy(out=nbf[:], in_=nst[:]), r=[B("nst")], w=[B("nbf")])
                            for dc in range(4):
                                pc_, pcb = PS[4 + dc % 2], B("ps", 4 + dc % 2)
                                S.op("pe", lambda e, dc=dc, kwi=kwi, c4=c4, pc_=pc_: e.matmul(pc_[:], lhsT=kw[kwi][:, dc * 128:(dc + 1) * 128], rhs=vch[:, c4, :], start=True, stop=True),
                                     r=[B("kw", kwi), B("vch")], w=[pcb])
                                S.op("dve", lambda e, dc=dc, c=c, pc_=pc_: e.scalar_tensor_tensor(out=Cst[:, dc, :], in0=Cst[:, dc, :], scalar=dec[:, c:c + 1], in1=pc_[:], op0=ALU.mult, op1=ALU.add),
                                     r=[B("Cst", dc), B("dec"), pcb], w=[B("Cst", dc)])
                                S.op("pool", lambda e, dc=dc: e.tensor_copy(out=Cbf[:, dc, :], in_=Cst[:, dc, :]), r=[B("Cst", dc)], w=[B("Cbf")])
            S.barrier()
        if on("p4"):
            with ExitStack() as ps_:
                sbp = lambda name, shape, dtype=F32: ps_.enter_context(nc.sbuf_tensor(name, list(shape), dtype))
                PS = [ps_.enter_context(nc.psum_tensor("p4ps%d" % i, [128, 512], F32)) for i in range(7)]
                PSB = ps_.enter_context(nc.psum_tensor("p4psb", [128, 512], BF16))
                yT = sbp("yT", [128, KC, SL], BF16)
                Wo = [sbp("Wo%d" % i, [128, KC, 256], BF16) for i in range(3)]
                hT = sbp("hT", [128, KC, SL])
                xc = [sbp("xc%d" % i, [128, SL]) for i in range(3)]
                sq = [sbp("sq4_%d" % i, [128, SL], BF16) for i in range(3)]
                rstd = sbp("rstd4", [128, SL])
                tmp = [sbp("tmp4_%d" % i, [128, SL]) for i in range(3)]
                u2f = [sbp("u2f%d" % i, [128, SL]) for i in range(3)]
                u2b = [sbp("u2b%d" % i, [128, SL], BF16) for i in range(3)]
                wrt = sbp("wrt", [128, KC, 36])
                brt = sbp("brt", [128, 36])
                L = sbp("L", [128, 36])
                r8 = sbp("r8", [128, 16])
                ohg = sbp("ohg", [128, 4])
                eg = sbp("eg", [128, 4])
                es_ = sbp("es_", [128, 8])
                e2 = sbp("e2", [128, 8])
                oh1 = sbp("oh1", [128, 8])
                oh2 = sbp("oh2", [128, 8])
                gw8 = sbp("gw8", [128, 8])
                gw32 = sbp("gw32", [128, 32])
                S.dma("sp", wrt[:], wr.rearrange("(c p) n -> p c n", p=128), w=[B("wrt")])
                S.dma("sp", brt[:], br[:], w=[B("brt")])
                woi = [0]
                for oi, so in enumerate(OWN):
                    osl = slice(oi * SL, (oi + 1) * SL)
                    S.dma("sp", yT[:], yT_scr[:, osl].rearrange("(c p) t -> p c t", p=128), r=[B("yT_scr")], w=[B("yT")])
                    for g in range(16):
                        k = woi[0] % 3
                        woi[0] += 1
                        S.dma("pool", Wo[k][:], w_out[:, g * 256:(g + 1) * 256].rearrange("(c p) n -> p c n", p=128), w=[B("Wo", k)])
                        for cc in range(2):
                            dc = g * 2 + cc
                            pt, pb = PS[dc % 4], B("ps", dc % 4)
                            for kc in range(KC):
                                S.op("pe", lambda e, kc=kc, cc=cc, pt=pt, k=k: e.matmul(pt[:], lhsT=Wo[k][:, kc, cc * 128:(cc + 1) * 128], rhs=yT[:, kc, :],
                                                                                  start=(kc == 0), stop=(kc == KC - 1)),
                                     r=[B("Wo", k), B("yT")], acc=pb, w=[pb], inc=(kc == KC - 1))
                            xi = dc % 3
                            S.dma("act", xc[xi][:], xs[dc * 128:(dc + 1) * 128, so * SL:(so + 1) * SL], w=[B("xc", xi)])
                            S.op("dve", lambda e, dc=dc, pt=pt, xi=xi: e.scalar_tensor_tensor(out=hT[:, dc, :], in0=pt[:], scalar=mod[:, 2, dc:dc + 1], in1=xc[xi][:], op0=ALU.mult, op1=ALU.add),
                                 r=[pb, B("mod"), B("xc", xi)], w=[B("hT", dc)])
                            S.dma("sp", hT_scr[dc * 128:(dc + 1) * 128, osl], hT[:, dc, :], r=[B("hT", dc)], w=[B("hT_scr")])
                    pt, pb = PS[4], B("ps", 4)
                    for kc in range(KC):
                        qi = kc % 3
                        S.op("act", lambda e, qi=qi, kc=kc: e.activation(out=sq[qi][:], in_=hT[:, kc, :], func=AF.Square), r=[B("hT", kc)], w=[B("sq4", qi)])
                        S.op("pe", lambda e, qi=qi, kc=kc: e.matmul(pt[:], lhsT=cm_b[:, ONE, :], rhs=sq[qi][:], start=(kc == 0), stop=(kc == KC - 1)),
                             r=[B("sq4", qi), B("cm_b")], acc=pb, w=[pb], inc=True)
                    S.op("dve", lambda e: e.tensor_scalar(out=rstd[:], in0=pt[:], scalar1=1.0 / D, scalar2=EPS, op0=ALU.mult, op1=ALU.add), r=[pb], w=[B("rstd4")])
                    S.op("act", lambda e: e.activation(out=rstd[:], in_=rstd[:], func=AF.Sqrt), r=[B("rstd4")], w=[B("rstd4")])
                    S.op("dve", lambda e: e.reciprocal(out=rstd[:], in_=rstd[:]), r=[B("rstd4")], w=[B("rstd4")])
                    lp, lpb = PS[5], B("ps", 5)
                    for kc in range(KC):
                        ti = kc % 3
                        S.op("dve", lambda e, ti=ti, kc=kc: e.tensor_tensor(out=tmp[ti][:], in0=hT[:, kc, :], in1=rstd[:], op=ALU.mult),
                             r=[B("hT", kc), B("rstd4")], w=[B("tmp4", ti)])
                        S.op("act", lambda e, ti=ti, kc=kc: e.activation(out=u2f[ti][:], in_=tmp[ti][:], func=AF.Identity, scale=AB[:, 2, kc:kc + 1], bias=AB[:, 3, kc:kc + 1]),
                             r=[B("tmp4", ti), B("AB")], w=[B("u2f", ti)])
                        S.op("pool", lambda e, ti=ti: e.tensor_copy(out=u2b[ti][:], in_=u2f[ti][:]), r=[B("u2f", ti)], w=[B("u2b", ti)])
                        S.dma("sp", u2T_scr[kc * 128:(kc + 1) * 128, osl], u2b[ti][:], r=[B("u2b", ti)], w=[B("u2T_scr")])
                        for tb in range(4):
                            S.op("pe", lambda e, ti=ti, kc=kc, tb=tb: e.matmul(PS[tb][:, 0:36], lhsT=u2f[ti][:, tb * 128:(tb + 1) * 128], rhs=wrt[:, kc, :],
                                                                           start=(kc == 0), stop=(kc == KC - 1)),
                                 r=[B("u2f", ti), B("wrt")], acc=B("ps", tb), w=[B("ps", tb)], inc=(tb == 3))
                    for tb in range(4):
                        dv = lambda fn, r, w: S.op("dve", fn, r=r, w=w)
                        RB = [B("rt")]
                        dv(lambda e, tb=tb: e.tensor_tensor(out=L[:], in0=PS[tb][:, 0:36], in1=brt[:], op=ALU.add), [B("ps", tb), B("brt")] + RB, RB)
                        dv(lambda e: e.tensor_reduce(out=r8[:, 0:1], in_=L[:, 0:4], axis=AX.X, op=ALU.max), RB, RB)
                        dv(lambda e: e.tensor_scalar(out=r8[:, 1:2], in0=r8[:, 0:1], scalar1=-1.0, scalar2=None, op0=ALU.mult), RB, RB)
                        S.op("act", lambda e: e.activation(out=eg[:], in_=L[:, 0:4], func=AF.Exp, bias=r8[:, 1:2]), r=RB, w=RB)
                        dv(lambda e: e.tensor_reduce(out=r8[:, 2:3], in_=eg[:], axis=AX.X, op=ALU.add), RB, RB)
                        dv(lambda e: e.reciprocal(out=r8[:, 3:4], in_=r8[:, 2:3]), RB, RB)
                        dv(lambda e: e.tensor_scalar(out=ohg[:], in0=L[:, 0:4], scalar1=r8[:, 0:1], scalar2=None, op0=ALU.is_equal), RB, RB)
                        dv(lambda e: e.tensor_scalar(out=es_[:], in0=L[:, 4:12], scalar1=ohg[:, 0:1], scalar2=None, op0=ALU.mult), RB, RB)
                        for g in range(1, 4):
                            dv(lambda e, g=g: e.scalar_tensor_tensor(out=es_[:], in0=L[:, 4 + g * 8: 12 + g * 8], scalar=ohg[:, g:g + 1], in1=es_[:], op0=ALU.mult, op1=ALU.add), RB, RB)
                        dv(lambda e: e.tensor_reduce(out=r8[:, 4:5], in_=es_[:], axis=AX.X, op=ALU.max), RB, RB)
                        dv(lambda e: e.tensor_scalar(out=oh1[:], in0=es_[:], scalar1=r8[:, 4:5], scalar2=None, op0=ALU.is_equal), RB, RB)
                        dv(lambda e: e.scalar_tensor_tensor(out=e2[:], in0=oh1[:], scalar=-1e30, in1=es_[:], op0=ALU.mult, op1=ALU.add), RB, RB)
                        dv(lambda e: e.tensor_reduce(out=r8[:, 5:6], in_=e2[:], axis=AX.X, op=ALU.max), RB, RB)
                        dv(lambda e: e.tensor_scalar(out=oh2[:], in0=e2[:], scalar1=r8[:, 5:6], scalar2=None, op0=ALU.is_equal), RB, RB)
                        dv(lambda e: e.tensor_tensor(out=r8[:, 6:7], in0=r8[:, 5:6], in1=r8[:, 4:5], op=ALU.subtract), RB, RB)
                        S.op("act", lambda e: e.activation(out=r8[:, 7:8], in_=r8[:, 6:7], func=AF.Exp), r=RB, w=RB)
                        dv(lambda e: e.tensor_scalar(out=r8[:, 8:9], in0=r8[:, 7:8], scalar1=1.0, scalar2=None, op0=ALU.add), RB, RB)
                        dv(lambda e: e.reciprocal(out=r8[:, 9:10], in_=r8[:, 8:9]), RB, RB)
                        dv(lambda e: e.tensor_tensor(out=r8[:, 10:11], in0=r8[:, 9:10], in1=r8[:, 3:4], op=ALU.mult), RB, RB)
                        dv(lambda e: e.tensor_tensor(out=r8[:, 11:12], in0=r8[:, 10:11], in1=r8[:, 7:8], op=ALU.mult), RB, RB)
                        dv(lambda e: e.tensor_scalar(out=gw8[:], in0=oh1[:], scalar1=r8[:, 10:11], scalar2=None, op0=ALU.mult), RB, RB)
                        dv(lambda e: e.scalar_tensor_tensor(out=gw8[:], in0=oh2[:], scalar=r8[:, 11:12], in1=gw8[:], op0=ALU.mult, op1=ALU.add), RB, RB)
                        for g in range(4):
                            dv(lambda e, g=g: e.tensor_scalar(out=gw32[:, g * 8:(g + 1) * 8], in0=gw8[:], scalar1=ohg[:, g:g + 1], scalar2=None, op0=ALU.mult), RB, RB)
                        tp, tpb = PS[6], B("ps", 6)
                        S.op("pe", lambda e: e.transpose(tp[0:32, 0:128], gw32[:], cm_f[:, IDF, :]), r=RB + [B("cm_f")], w=[tpb])
                        dv(lambda e, tb=tb, oi=oi: e.tensor_copy(out=gwT[:, oi * SL + tb * 128: oi * SL + (tb + 1) * 128], in_=tp[0:32, 0:128]), [tpb], [B("gwT")])
            S.barrier()
        if "gwT" in dbg:
            S.dma("sp", dbg["gwT"], gwT[:], r=[B("gwT")])

        if on("p5"):
            for tile_ in range(2):
                tsl = slice(tile_ * 1024, (tile_ + 1) * 1024)
                with ExitStack() as ps_:
                    sbp = lambda name, shape, dtype=F32: ps_.enter_context(nc.sbuf_tensor(name + "_t%d" % tile_, list(shape), dtype))
                    PS = [ps_.enter_context(nc.psum_tensor("p5aps%d_%d" % (tile_, i), [128, 512], F32)) for i in range(7)]
                    u2 = sbp("u2", [128, KC, 1024], BF16)
                    Wb = [sbp("Wb%d" % i, [128, KC, 512], BF16) for i in range(3)]
                    gwb = [sbp("gwb%d" % i, [128, 1024]) for i in range(2)]
                    gm = [sbp("gm%d" % i, [32, 1024]) for i in range(2)]
                    sgl = [sbp("sgl%d" % i, [128, SL]) for i in range(2)]
                    tu = [sbp("tu%d" % i, [128, SL]) for i in range(2)]
                    actS = [sbp("actS%d" % i, [128, 2, 1024], BF16) for i in range(2)]
                    S.dma("sp", u2[:], u2T_scr[:, tsl].rearrange("(c p) t -> p c t", p=128), r=[B("u2T_scr")], w=[B("u2")])
                    rot = [0, 0, 0]
                    for ex in range(NEXP):
                        gi = ex % 2
                        S.op("dve", lambda e, ex=ex, gi=gi: e.tensor_scalar(out=gm[gi][:], in0=gwT[:, tsl], scalar1=cm_f[0:32, IDF, ex:ex + 1], scalar2=None, op0=ALU.mult),
                             r=[B("gwT"), B("cm_f")], w=[B("gm", gi)])
                        for half in range(2):
                            bp, bpb = PS[6], B("ps", 6)
                            S.op("pe", lambda e, gi=gi, half=half: e.matmul(bp[:], lhsT=cm_f[0:32, ONE, :], rhs=gm[gi][:, half * SL:(half + 1) * SL], start=True, stop=True),
                                 r=[B("gm", gi), B("cm_f")], w=[bpb])
                            S.op("act", lambda e, gi=gi, half=half: e.activation(out=gwb[gi][:, half * SL:(half + 1) * SL], in_=bp[:], func=AF.Copy), r=[bpb], w=[B("gwb", gi)])
                        for fh in range(2):
                            kg = rot[0] % 3
                            rot[0] += 1
                            ku = rot[0] % 3
                            rot[0] += 1
                            S.dma("pool", Wb[kg][:], wg[ex, :, fh * 512:(fh + 1) * 512].rearrange("(c p) n -> p c n", p=128), w=[B("Wb", kg)])
                            S.dma("pool", Wb[ku][:], wu[ex, :, fh * 512:(fh + 1) * 512].rearrange("(c p) n -> p c n", p=128), w=[B("Wb", ku)])
                            for fpair in range(2):
                                ai = rot[1] % 2
                                rot[1] += 1
                                for f2 in range(2):
                                    f4 = fpair * 2 + f2
                                    for half in range(2):
                                        r3 = rot[2] % 3
                                        t2 = rot[2] % 2
                                        rot[2] += 1
                                        gp, gpb = PS[2 * r3], B("ps", 2 * r3)
                                        up, upb = PS[2 * r3 + 1], B("ps", 2 * r3 + 1)
                                        hs = slice(half * SL, (half + 1) * SL)
                                        for kc in range(KC):
                                            S.op("pe", lambda e, kc=kc, kg=kg, gp=gp, f4=f4, hs=hs: e.matmul(gp[:], lhsT=Wb[kg][:, kc, f4 * 128:(f4 + 1) * 128], rhs=u2[:, kc, hs], start=(kc == 0), stop=(kc == KC - 1)),
                                                 r=[B("Wb", kg), B("u2")], acc=gpb, w=[gpb], inc=(kc == KC - 1))
                                        for kc in range(KC):
                                            S.op("pe", lambda e, kc=kc, ku=ku, up=up, f4=f4, hs=hs: e.matmul(up[:], lhsT=Wb[ku][:, kc, f4 * 128:(f4 + 1) * 128], rhs=u2[:, kc, hs], start=(kc == 0), stop=(kc == KC - 1)),
                                                 r=[B("Wb", ku), B("u2")], acc=upb, w=[upb], inc=(kc == KC - 1))
                                        S.op("act", lambda e, t2=t2, gp=gp: e.activation(out=sgl[t2][:], in_=gp[:], func=AF.Silu), r=[gpb], w=[B("sgl", t2)])
                                        S.op("dve", lambda e, t2=t2, up=up, gi=gi, hs=hs: e.tensor_tensor(out=tu[t2][:], in0=up[:], in1=gwb[gi][:, hs], op=ALU.mult), r=[upb, B("gwb", gi)], w=[B("tu", t2)])
                                        S.op("pool", lambda e, t2=t2, ai=ai, f2=f2, hs=hs: e.tensor_tensor(out=actS[ai][:, f2, hs], in0=tu[t2][:], in1=sgl[t2][:], op=ALU.mult),
                                             r=[B("tu", t2), B("sgl", t2)], w=[B("actS", ai)])
                                r0 = (fh * 4 + fpair * 2) * 128
                                S.dma("sp", act_scr[ex, r0:r0 + 256, :].rearrange("(c p) t -> p c t", p=128), actS[ai][:], r=[B("actS", ai)], w=[B("act_scr")])
                S.barrier()
                with ExitStack() as ps_:
                    sbp = lambda name, shape, dtype=F32: ps_.enter_context(nc.sbuf_tensor(name + "_t%d" % tile_, list(shape), dtype))
                    PS = [ps_.enter_context(nc.psum_tensor("p5bps%d_%d" % (tile_, i), [128, 512], F32)) for i in range(7)]
                    acc = sbp("acc", [128, KC, 1024])
                    actT = sbp("actT", [128, 8, 1024], BF16)
                    Wd = [sbp("Wd%d" % i, [128, 8, 512], BF16) for i in range(2)]
                    hx = [sbp("hx%d" % i, [128, SL]) for i in range(2)]
                    sq = [sbp("sq5_%d" % i, [128, SL], BF16) for i in range(2)]
                    rstd = sbp("rstd5", [128, SL])
                    tmp = [sbp("tmp5_%d" % i, [128, SL]) for i in range(2)]
                    ot = [sbp("ot%d" % i, [128, SL]) for i in range(2)]
                    S.op("pool", lambda e: e.memset(acc[:], 0.0), w=[B("acc", d_, h_) for d_ in range(KC) for h_ in range(2)])
                    cnt = [0, 0]
                    for ex in range(NEXP):
                        S.dma("sp", actT[:], act_scr[ex].rearrange("(c p) t -> p c t", p=128), r=[B("act_scr")], w=[B("actT")])
                        for dg in range(8):
                            k = cnt[0] % 2
                            cnt[0] += 1
                            S.dma("pool", Wd[k][:], wd[ex, :, dg * 512:(dg + 1) * 512].rearrange("(c p) n -> p c n", p=128), w=[B("Wd", k)])
                            for d4 in range(4):
                                dc = dg * 4 + d4
                                for half in range(2):
                                    pi = cnt[1] % 6
                                    cnt[1] += 1
                                    dp, dpb = PS[pi], B("ps", pi)
                                    hs = slice(half * SL, (half + 1) * SL)
                                    for fc in range(8):
                                        S.op("pe", lambda e, fc=fc, k=k, d4=d4, dp=dp, hs=hs: e.matmul(dp[:], lhsT=Wd[k][:, fc, d4 * 128:(d4 + 1) * 128], rhs=actT[:, fc, hs], start=(fc == 0), stop=(fc == 7)),
                                             r=[B("Wd", k), B("actT")], acc=dpb, w=[dpb], inc=(fc == 7))
                                    S.op("dve", lambda e, dc=dc, dp=dp, hs=hs: e.tensor_tensor(out=acc[:, dc, hs], in0=acc[:, dc, hs], in1=dp[:], op=ALU.add),
                                         r=[dpb, B("acc", dc, half)], w=[B("acc", dc, half)])
                    for half in range(2):
                        oi = 2 * tile_ + half
                        osl = slice(oi * SL, (oi + 1) * SL)
                        hs = slice(half * SL, (half + 1) * SL)
                        pt, pb = PS[6], B("ps", 6)
                        for kc in range(KC):
                            hi = kc % 2
                            S.dma("sp", hx[hi][:], hT_scr[kc * 128:(kc + 1) * 128, osl], r=[B("hT_scr")], w=[B("hx", hi)])
                            S.op("dve", lambda e, kc=kc, hi=hi, hs=hs: e.scalar_tensor_tensor(out=acc[:, kc, hs], in0=acc[:, kc, hs], scalar=mod[:, 5, kc:kc + 1], in1=hx[hi][:], op0=ALU.mult, op1=ALU.add),
                                 r=[B("acc", kc, half), B("mod"), B("hx", hi)], w=[B("acc", kc, half)])
                            qi = kc % 2
                            S.op("act", lambda e, qi=qi, kc=kc, hs=hs: e.activation(out=sq[qi][:], in_=acc[:, kc, hs], func=AF.Square), r=[B("acc", kc, half)], w=[B("sq5", qi)])
                            S.op("pe", lambda e, qi=qi, kc=kc: e.matmul(pt[:], lhsT=cm_b[:, ONE, :], rhs=sq[qi][:], start=(kc == 0), stop=(kc == KC - 1)),
                                 r=[B("sq5", qi), B("cm_b")], acc=pb, w=[pb], inc=True)
                        S.op("dve", lambda e: e.tensor_scalar(out=rstd[:], in0=pt[:], scalar1=1.0 / D, scalar2=EPS, op0=ALU.mult, op1=ALU.add), r=[pb], w=[B("rstd5")])
                        S.op("act", lambda e: e.activation(out=rstd[:], in_=rstd[:], func=AF.Sqrt), r=[B("rstd5")], w=[B("rstd5")])
                        S.op("dve", lambda e: e.reciprocal(out=rstd[:], in_=rstd[:]), r=[B("rstd5")], w=[B("rstd5")])
                        for kc in range(KC):
                            ti = kc % 2
                            S.op("dve", lambda e, ti=ti, kc=kc, hs=hs: e.tensor_tensor(out=tmp[ti][:], in0=acc[:, kc, hs], in1=rstd[:], op=ALU.mult),
                                 r=[B("acc", kc, half), B("rstd5")], w=[B("tmp5", ti)])
                            S.op("act", lambda e, ti=ti, kc=kc: e.activation(out=ot[ti][:], in_=tmp[ti][:], func=AF.Identity, scale=AB[:, 4, kc:kc + 1], bias=AB[:, 5, kc:kc + 1]),
                                 r=[B("tmp5", ti), B("AB")], w=[B("ot", ti)])
                            S.dma("sp", outT[kc * 128:(kc + 1) * 128, osl], ot[ti][:], r=[B("ot", ti)], w=[B("outT")])
                S.barrier()
        for nm in debug:
            if nm in SCR:
                S.dma("sp", dbg[nm], SCR[nm][0], r=[B(nm)])
        S.barrier()
    return nc, S, EXT


def _pc(v):
    return np.ascontiguousarray(np.asarray(v, np.float32).reshape(-1, 128).T)


def kernel(x, c, norm1_w, w_in, conv_w, ml_gate_bias, ml_norm_w, w_out, norm2_w,
           w_router_group, b_router_group, w_router_expert, b_router_expert,
           w_exp_gate, w_exp_up, w_exp_down, w_ada, b_ada, final_norm_w,
           w_ada_final, b_ada_final, _phases=("all",), _debug=(), _cores=None):
    f = lambda a: np.ascontiguousarray(np.asarray(a, dtype=np.float32))
    x = f(x)
    nc, S, EXT = build(phases=_phases, debug=_debug)
    j_ = np.arange(128)
    cmat = np.zeros((128, 5, 128), np.float32)
    cmat[:, 0, :] = np.eye(128)
    cmat[:, 1, :] = (j_[:, None] >= j_[None, :])
    cmat[:, 2, :] = (j_[:, None] < j_[None, :])
    cmat[:, 3, :] = (j_[:, None] <= j_[None, :])
    cmat[:, 4, :] = 1.0
    t_ = np.arange(SL)
    cmask = np.zeros((128, 4, SL), np.float32)
    for m in range(4):
        cmask[:, m, :] = ((j_[:, None] + 128 * m) < t_[None, :])
    csel = np.zeros((32, NEXP, 128), np.float32)
    for e in range(NEXP):
        csel[e, e, :] = 1.0
    vecs = np.zeros((128, 8, KC), np.float32)
    vecs[:, 0, :] = _pc(norm1_w[0])
    vecs[:, 1, :] = _pc(norm2_w[0])
    vecs[:, 2, :] = _pc(final_norm_w)
    bada = np.zeros((128, 8, KC), np.float32)
    ba = np.asarray(b_ada[0], np.float32)
    for v in range(6):
        bada[:, v, :] = _pc(ba[v * D:(v + 1) * D])
    baf = np.asarray(b_ada_final, np.float32)
    for v in range(2):
        bada[:, 6 + v, :] = _pc(baf[v * D:(v + 1) * D])
    cw = np.asarray(conv_w[0], np.float32)
    convw = np.stack([_pc(cw[i]) for i in range(4)], axis=1)
    gbias = np.ascontiguousarray(np.broadcast_to(np.asarray(ml_gate_bias[0], np.float32)[None, :], (128, 8)))
    mlnw = np.ascontiguousarray(np.broadcast_to(np.asarray(ml_norm_w[0], np.float32)[None, :], (128, 2048)))
    wr = np.ascontiguousarray(np.concatenate([np.asarray(w_router_group[0], np.float32), np.asarray(w_router_expert[0], np.float32)], axis=1))
    brv = np.concatenate([np.asarray(b_router_group[0], np.float32), np.asarray(b_router_expert[0], np.float32)])
    br = np.ascontiguousarray(np.broadcast_to(brv[None, :], (128, 36)))
    shared = dict(vecs=vecs, bada=bada, w_in=f(w_in[0]), w_out=f(w_out[0]), w_ada=f(w_ada[0]), w_adaf=f(w_ada_final),
                  convw=np.ascontiguousarray(convw), gbias=gbias, mlnw=mlnw, wr=wr, br=br,
                  wg=f(w_exp_gate[0]), wu=f(w_exp_up[0]), wd=f(w_exp_down[0]), cmat=cmat, cmask=cmask)
    in_maps = []
    for core in range(8):
        b, j = core // 4, core % 4
        xs = np.zeros((D, NSLOT * SL), np.float32)
        sv = np.zeros((128, NSLOT), np.float32)
        for s in range(NSLOT):
            g = s + j - 3
            if g >= 0:
                xs[:, s * SL:(s + 1) * SL] = x[b, g * SL:(g + 1) * SL, :].T
                sv[:, s] = 1.0
        m = dict(shared)
        m["xs"] = xs
        m["slotvalid"] = sv
        m["cpc"] = _pc(c[b])
        in_maps.append({k: v for k, v in m.items() if k in EXT})
    if _cores is not None:
        in_maps = [in_maps[i] for i in _cores]
        return run_bass_kernel_spmd(nc, in_maps, core_ids=list(range(len(_cores))))
    res = run_bass_kernel_spmd(nc, in_maps, core_ids=list(range(8)))
    out = np.empty((2, 8192, D), np.float32)
    for core in range(8):
        b, j = core // 4, core % 4
        oT = np.asarray(res.results[core]["outT"])
        for oi in range(4):
            g = j + 4 * oi
            out[b, g * SL:(g + 1) * SL, :] = oT[:, oi * SL:(oi + 1) * SL].T
    return out
```
